# Optimizing a Trainium2 kernel written in Bass

```python
import jax, jax.numpy as jnp
from jax import lax
import numpy as np

D_MODEL = 1024
BATCH = 16
SEQ = 256
DEPTH = 4
DEC_BATCH = 4
DEC_SEQ = 2048
PAST_LEN = 256

GRID_W = 64
N_EVEN = (DEPTH + 1) // 2
N_ODD = DEPTH // 2
FNET_WIDTH = D_MODEL // 2
FNET_GROUPS = 4
FNET_GROUP_DIM = FNET_WIDTH // FNET_GROUPS
GLA_HEADS = 4
GLA_DK = D_MODEL // 4
GLA_DV = D_MODEL // 2
GLA_DK_HEAD = GLA_DK // GLA_HEADS
GLA_DV_HEAD = GLA_DV // GLA_HEADS
GLA_GATE_RANK = 16
GLA_TAU = 16.0
GLA_CHUNK = 64
EVEN_SPLITS = (FNET_WIDTH, FNET_WIDTH + GLA_DK, FNET_WIDTH + 2 * GLA_DK, FNET_WIDTH + 2 * GLA_DK + GLA_DV, FNET_WIDTH + 2 * GLA_DK + 2 * GLA_DV)
EVEN_IN = FNET_WIDTH + 2 * GLA_DK + 2 * GLA_DV + 2 * GLA_GATE_RANK
ATT_HEADS = 8
ATT_KV_HEADS = 2
HEAD_DIM = D_MODEL // ATT_HEADS
ROPE_THETA = 10000.0
ROPE_PAIRS_PER_AXIS = HEAD_DIM // 4
Q_BLOCK = 128
ODD_SPLITS = (ATT_HEADS * HEAD_DIM, (ATT_HEADS + ATT_KV_HEADS) * HEAD_DIM)
ODD_IN = (ATT_HEADS + 2 * ATT_KV_HEADS) * HEAD_DIM
N_EXPERTS = 32
TOP_K = 4
D_FF = D_MODEL
SWIGLU_LIMIT = 7.0
SWIGLU_ALPHA = 1.702
LN_EPS = 1e-5
RMS_EPS = 1e-6
DEEPNORM_ALPHA = (2.0 * DEPTH) ** 0.25
DEEPNORM_BETA = (8.0 * DEPTH) ** -0.25

kernel_name = 'hybrid_fnet_gla_gqa_moe_diffusion_step'


def layer_norm(x, g=None, b=None):
    xf = x.astype(jnp.float32)
    mu = jnp.mean(xf, axis=-1, keepdims=True)
    var = jnp.mean(jnp.square(xf - mu), axis=-1, keepdims=True)
    y = (xf - mu) * lax.rsqrt(var + LN_EPS)
    if g is not None:
        y = y * g.astype(jnp.float32) + b.astype(jnp.float32)
    return y.astype(x.dtype)


def rms_norm(x, g):
    xf = x.astype(jnp.float32)
    y = xf * lax.rsqrt(jnp.mean(jnp.square(xf), axis=-1, keepdims=True) + RMS_EPS)
    return (y * g.astype(jnp.float32)).astype(x.dtype)


def modulation(cmod, w_mod_l, b_mod_l):
    m = jax.nn.silu(cmod) @ w_mod_l + b_mod_l
    return jnp.split(m[:, None, :], 6, axis=-1)


def axial_rope(n_tok):
    n_rows = n_tok // GRID_W
    row = jnp.broadcast_to(jnp.arange(n_rows, dtype=jnp.float32)[:, None], (n_rows, GRID_W)).reshape(-1)
    col = jnp.broadcast_to(jnp.arange(GRID_W, dtype=jnp.float32)[None, :], (n_rows, GRID_W)).reshape(-1)
    freqs = ROPE_THETA ** (-jnp.arange(ROPE_PAIRS_PER_AXIS, dtype=jnp.float32) / ROPE_PAIRS_PER_AXIS)
    ang = jnp.concatenate([row[:, None] * freqs, col[:, None] * freqs], axis=-1)
    return jnp.cos(ang), jnp.sin(ang)


def apply_rope(x, cos, sin):
    b, t, h, d = x.shape
    xp = x.astype(jnp.float32).reshape(b, t, h, d // 2, 2)
    x0, x1 = xp[..., 0], xp[..., 1]
    c = cos[None, :, None, :]
    s = sin[None, :, None, :]
    out = jnp.stack([x0 * c - x1 * s, x0 * s + x1 * c], axis=-1).reshape(b, t, h, d)
    return out.astype(x.dtype)


def block_attention(q, k, v):
    b, tq, h, hd = q.shape
    g = h // ATT_KV_HEADS
    nb = tq // Q_BLOCK
    qb = q.reshape(b, nb, Q_BLOCK, ATT_KV_HEADS, g, hd).transpose(1, 0, 2, 3, 4, 5)
    scale = hd ** -0.5

    def one_block(qblk):
        s = jnp.einsum('bqkgd,bskd->bkgqs', qblk, k, preferred_element_type=jnp.float32) * scale
        p = jax.nn.softmax(s, axis=-1)
        return jnp.einsum('bkgqs,bskd->bqkgd', p.astype(v.dtype), v)

    o = lax.map(one_block, qb)
    return o.transpose(1, 0, 2, 3, 4, 5).reshape(b, tq, h, hd)


def gla_chunk_scan(q, k, v, logg, s0):
    b, t, h, _ = q.shape
    dv = v.shape[-1]
    n = t // GLA_CHUNK

    def to_chunks(a):
        return a.reshape(b, n, GLA_CHUNK, h, a.shape[-1]).transpose(1, 0, 3, 2, 4)

    causal = jnp.tril(jnp.ones((GLA_CHUNK, GLA_CHUNK), dtype=bool))[:, :, None]

    def step(s, inp):
        qc, kc, vc, gc = inp
        cum = jnp.cumsum(gc, axis=-2)
        diff = cum[:, :, :, None, :] - cum[:, :, None, :, :]
        decay = jnp.exp(jnp.where(causal, diff, -jnp.inf))
        att = jnp.einsum('bhid,bhjd,bhijd->bhij', qc, kc, decay)
        o = att @ vc + jnp.einsum('bhid,bhde->bhie', qc * jnp.exp(cum), s)
        last = cum[:, :, -1:, :]
        s_new = jnp.exp(last[:, :, 0, :])[..., None] * s + jnp.einsum('bhjd,bhje->bhde', kc * jnp.exp(last - cum), vc)
        return s_new, o

    s_fin, o = lax.scan(step, s0.astype(jnp.float32), (to_chunks(q), to_chunks(k), to_chunks(v), to_chunks(logg)))
    o = o.transpose(1, 0, 3, 2, 4).reshape(b, t, h, dv)
    return o, s_fin


def even_mixer(h, w_in, w_a2, b_a, gla_norm_g, w_out, s0):
    b, t, _ = h.shape
    proj = h @ w_in
    u_f, q, k, v, g, a_lr = jnp.split(proj, EVEN_SPLITS, axis=-1)
    uf = u_f.astype(jnp.float32).reshape(b, t, FNET_GROUPS, FNET_GROUP_DIM)
    f_out = jnp.real(jnp.fft.fft2(uf, axes=(1, 3), norm='ortho')).reshape(b, t, FNET_WIDTH)
    qf = q.astype(jnp.float32).reshape(b, t, GLA_HEADS, GLA_DK_HEAD) * (GLA_DK_HEAD ** -0.5)
    kf = k.astype(jnp.float32).reshape(b, t, GLA_HEADS, GLA_DK_HEAD)
    vf = v.astype(jnp.float32).reshape(b, t, GLA_HEADS, GLA_DV_HEAD)
    a_lr = a_lr.reshape(b, t, 2, GLA_GATE_RANK)
    gate_logits = jnp.einsum('btzr,zre->btze', a_lr, w_a2) + b_a
    logg = (jax.nn.log_sigmoid(gate_logits.astype(jnp.float32)) / GLA_TAU).reshape(b, t, 2, GLA_HEADS, GLA_DK_HEAD)
    o_f, s_f = gla_chunk_scan(qf, kf, vf, logg[:, :, 0], s0[:, 0])
    flip = lambda a: jnp.flip(a, axis=1)
    o_b, s_b = gla_chunk_scan(flip(qf), flip(kf), flip(vf), flip(logg[:, :, 1]), s0[:, 1])
    o = o_f + flip(o_b)
    o = rms_norm(o, gla_norm_g).reshape(b, t, GLA_DV) * jax.nn.silu(g.astype(jnp.float32))
    mix = jnp.concatenate([f_out, o], axis=-1).astype(h.dtype)
    return mix @ w_out, jnp.stack([s_f, s_b], axis=1)


def odd_qkv(h, w_qkv, q_norm_g, k_norm_g):
    b, t, _ = h.shape
    q, k, v = jnp.split(h @ w_qkv, ODD_SPLITS, axis=-1)
    q = rms_norm(q.reshape(b, t, ATT_HEADS, HEAD_DIM), q_norm_g)
    k = rms_norm(k.reshape(b, t, ATT_KV_HEADS, HEAD_DIM), k_norm_g)
    v = v.reshape(b, t, ATT_KV_HEADS, HEAD_DIM)
    return q, k, v


def moe(h, w_router, b_router, w_gu, b_gu, w_down, b_down):
    b, t, d = h.shape
    xt = h.reshape(b * t, d)
    logits = (xt @ w_router + b_router).astype(jnp.float32)
    top_v, top_i = lax.top_k(logits, TOP_K)
    top_w = jax.nn.softmax(top_v, axis=-1)
    combine = jnp.einsum('nk,nke->ne', top_w, jax.nn.one_hot(top_i, N_EXPERTS, dtype=jnp.float32))
    y = jnp.zeros((b * t, d), jnp.float32)
    for e in range(N_EXPERTS):
        gu = xt @ w_gu[e] + b_gu[e]
        gate = jnp.minimum(gu[:, :D_FF], SWIGLU_LIMIT)
        up = jnp.clip(gu[:, D_FF:], -SWIGLU_LIMIT, SWIGLU_LIMIT)
        act = (up + 1.0) * gate * jax.nn.sigmoid(SWIGLU_ALPHA * gate)
        y = y + combine[:, e:e + 1] * (act @ w_down[e] + b_down[e])
    return y.reshape(b, t, d).astype(h.dtype)


def trunk(x, cmod, p, gla_init=None, ctx_k=None, ctx_v=None):
    latent = gla_init is not None
    b, t, _ = x.shape
    if latent:
        cos, sin = axial_rope(t)
    new_s, new_k, new_v = [], [], []
    for l in range(DEPTH):
        i = l // 2
        sh1, sc1, g1, sh2, sc2, g2 = modulation(cmod, p['w_mod'][l], p['b_mod'][l])
        h = layer_norm(x) * (1 + sc1) + sh1
        if l % 2 == 0:
            if latent:
                s0 = gla_init[:, i]
            else:
                s0 = jnp.zeros((b, 2, GLA_HEADS, GLA_DK_HEAD, GLA_DV_HEAD), jnp.float32)
            out, s_fin = even_mixer(h, p['w_in_even'][i], p['w_a2'][i], p['b_a'][i], p['gla_norm_g'][i], p['w_out_even'][i], s0)
            if not latent:
                new_s.append(s_fin.astype(x.dtype))
        else:
            q, k, v = odd_qkv(h, p['w_qkv'][i], p['q_norm_g'][i], p['k_norm_g'][i])
            if latent:
                q = apply_rope(q, cos, sin)
                k_rot = apply_rope(k, cos, sin)
                keys = jnp.concatenate([ctx_k[:, i].astype(k.dtype), k_rot], axis=1)
                vals = jnp.concatenate([ctx_v[:, i].astype(v.dtype), v], axis=1)
                o = block_attention(q, keys, vals)
            else:
                o = block_attention(q, k, v)
                new_k.append(k)
                new_v.append(v)
            out = o.reshape(b, t, D_MODEL) @ p['w_o'][i]
        x = layer_norm(DEEPNORM_ALPHA * x + g1 * out, p['ln_g'][l, 0], p['ln_b'][l, 0])
        h = layer_norm(x) * (1 + sc2) + sh2
        f = moe(h, p['w_router'][l], p['b_router'][l], p['w_gate_up'][l], p['b_gate_up'][l], p['w_down'][l], p['b_down'][l])
        x = layer_norm(DEEPNORM_ALPHA * x + g2 * f, p['ln_g'][l, 1], p['ln_b'][l, 1])
    return x, new_s, new_k, new_v


def setup_inputs(seed: int = 0) -> dict:
    key = jax.random.key(seed)
    ks = jax.random.split(key, 32)
    nrm = lambda k, shape, s: jax.random.normal(k, shape, jnp.float32) * s
    d = D_MODEL
    return {
        'x_prompt': nrm(ks[0], (BATCH, SEQ, d), 1.0),
        'x_sample': nrm(ks[1], (DEC_BATCH, DEC_SEQ, d), 1.0),
        'state_gla': nrm(ks[2], (DEC_BATCH, N_EVEN, 2, GLA_HEADS, GLA_DK_HEAD, GLA_DV_HEAD), 0.3),
        'cache_k': nrm(ks[3], (DEC_BATCH, N_ODD, PAST_LEN, ATT_KV_HEADS, HEAD_DIM), 1.0),
        'cache_v': nrm(ks[4], (DEC_BATCH, N_ODD, PAST_LEN, ATT_KV_HEADS, HEAD_DIM), 1.0),
        'c': nrm(ks[5], (DEC_BATCH, d), 1.0),
        'c_ctx': nrm(ks[6], (d,), 1.0),
        'w_mod': nrm(ks[7], (DEPTH, d, 6 * d), 0.5 * d ** -0.5),
        'b_mod': nrm(ks[8], (DEPTH, 6 * d), 0.01),
        'ln_g': 1.0 + nrm(ks[9], (DEPTH, 2, d), 0.02),
        'ln_b': nrm(ks[10], (DEPTH, 2, d), 0.02),
        'w_in_even': nrm(ks[11], (N_EVEN, d, EVEN_IN), d ** -0.5),
        'w_a2': nrm(ks[12], (N_EVEN, 2, GLA_GATE_RANK, GLA_DK), GLA_GATE_RANK ** -0.5),
        'b_a': nrm(ks[13], (N_EVEN, 2, GLA_DK), 0.1),
        'gla_norm_g': 1.0 + nrm(ks[14], (N_EVEN, GLA_DV_HEAD), 0.02),
        'w_out_even': nrm(ks[15], (N_EVEN, d, d), DEEPNORM_BETA * d ** -0.5),
        'w_qkv': nrm(ks[16], (N_ODD, d, ODD_IN), d ** -0.5),
        'q_norm_g': 1.0 + nrm(ks[17], (N_ODD, HEAD_DIM), 0.02),
        'k_norm_g': 1.0 + nrm(ks[18], (N_ODD, HEAD_DIM), 0.02),
        'w_o': nrm(ks[19], (N_ODD, d, d), DEEPNORM_BETA * d ** -0.5),
        'w_router': nrm(ks[20], (DEPTH, d, N_EXPERTS), d ** -0.5),
        'b_router': nrm(ks[21], (DEPTH, N_EXPERTS), 0.01),
        'w_gate_up': nrm(ks[22], (DEPTH, N_EXPERTS, d, 2 * D_FF), d ** -0.5),
        'b_gate_up': nrm(ks[23], (DEPTH, N_EXPERTS, 2 * D_FF), 0.01),
        'w_down': nrm(ks[24], (DEPTH, N_EXPERTS, D_FF, d), DEEPNORM_BETA * D_FF ** -0.5),
        'b_down': nrm(ks[25], (DEPTH, N_EXPERTS, d), 0.01),
    }


def reference(x_prompt, x_sample, state_gla, cache_k, cache_v, c, c_ctx, w_mod, b_mod, ln_g, ln_b,
              w_in_even, w_a2, b_a, gla_norm_g, w_out_even, w_qkv, q_norm_g, k_norm_g, w_o,
              w_router, b_router, w_gate_up, b_gate_up, w_down, b_down):
    p = dict(w_mod=w_mod, b_mod=b_mod, ln_g=ln_g, ln_b=ln_b, w_in_even=w_in_even, w_a2=w_a2, b_a=b_a,
             gla_norm_g=gla_norm_g, w_out_even=w_out_even, w_qkv=w_qkv, q_norm_g=q_norm_g,
             k_norm_g=k_norm_g, w_o=w_o, w_router=w_router, b_router=b_router, w_gate_up=w_gate_up,
             b_gate_up=b_gate_up, w_down=w_down, b_down=b_down)
    y_prompt, s_list, k_list, v_list = trunk(x_prompt, c_ctx[None, :], p)
    new_state_gla = jnp.stack(s_list, axis=1)
    new_cache_k = jnp.stack(k_list, axis=1)
    new_cache_v = jnp.stack(v_list, axis=1)
    y_sample, _, _, _ = trunk(x_sample, c, p, state_gla, cache_k, cache_v)
    return (y_prompt, y_sample, new_state_gla, new_cache_k, new_cache_v)
```

```python
import numpy as np
import ml_dtypes
import concourse.bass as bass
import concourse.mybir as mybir
from concourse.bass_utils import run_bass_kernel_spmd

F32 = mybir.dt.float32
BF16 = mybir.dt.bfloat16
AF = mybir.ActivationFunctionType
ALU = mybir.AluOpType
AX = mybir.AxisListType

D = 1024
KC = 8
T = 2048
TT = 512
NT = 4
NB = 16
DEPTH = 4
NE = 32
ALPHA = (2.0 * DEPTH) ** 0.25
NEG_BIG = -30000.0
ARENA_COLS = 52224
SAME_SYNC = True
NDS = 24

ENGS = ('pe', 'act', 'dve', 'pool', 'sp')
BLOCK_NAME = {'pe': 'tensor', 'act': 'scalar', 'dve': 'vector', 'pool': 'gpsimd', 'sp': 'sync'}


class Op:
    __slots__ = ('eng', 'fn', 'deps', 'is_dma', 'sem', 'val', 'prev')

    def __init__(self, eng, fn, deps, is_dma):
        self.eng = eng
        self.fn = fn
        self.deps = deps
        self.is_dma = is_dma
        self.sem = None
        self.val = None
        self.prev = 0


class Sync:
    def __init__(self, nc, stack):
        self.nc = nc
        self.eng_sem = {e: stack.enter_context(nc.semaphore('s_' + e)) for e in ('pe', 'act', 'dve', 'pool')}
        self.eng_cnt = {e: 0 for e in self.eng_sem}
        self.dma_sems = {q: [stack.enter_context(nc.semaphore('d_%s%d' % (q, i))) for i in range(NDS)]
                         for q in ('sp', 'pool')}
        self.dma_cnt = {q: [0] * NDS for q in ('sp', 'pool')}
        self.dma_rr = {q: 0 for q in ('sp', 'pool')}
        self.final_waits = []


class Blk:
    def __init__(self, nc, sync, ps):
        self.nc = nc
        self.sync = sync
        self.ops = []
        self.lw = {}
        self.rd = {}
        self.ps = ps
        self.bank_rr = 0

    def bank(self):
        b = self.bank_rr % 8
        self.bank_rr += 1
        return b

    def add(self, eng, fn, rd=(), wr=(), is_dma=False):
        idx = len(self.ops)
        deps = set()
        for k in rd:
            w = self.lw.get(k)
            if w is not None:
                deps.add(w)
        for k in wr:
            w = self.lw.get(k)
            if w is not None:
                deps.add(w)
            for r in self.rd.get(k, ()):
                deps.add(r)
        for k in rd:
            self.rd.setdefault(k, []).append(idx)
        for k in wr:
            self.lw[k] = idx
            self.rd[k] = []
        last = {}
        keep = []
        for dd in deps:
            od = self.ops[dd]
            if od.is_dma:
                keep.append(dd)
            elif last.get(od.eng, -1) < dd:
                last[od.eng] = dd
        keep.extend(last.values())
        self.ops.append(Op(eng, fn, sorted(keep), is_dma))
        return idx

    def mm(self, out, lhsT, rhs, start, stop, rd, wr):
        self.add('pe', lambda e: e.matmul(out, lhsT=lhsT, rhs=rhs, start=start, stop=stop), rd, wr)

    def tr(self, out, in_, ident, rd, wr):
        self.add('pe', lambda e: e.transpose(out, in_, ident), rd, wr)

    def act(self, out, in_, func, rd, wr, bias=None, scale=None, accum_out=None):
        kw = {}
        if bias is not None:
            kw['bias'] = bias
        if scale is not None:
            kw['scale'] = scale
        if accum_out is not None:
            kw['accum_out'] = accum_out
        self.add('act', lambda e: e.activation(out=out, in_=in_, func=func, **kw), rd, wr)

    def ts(self, eng, out, in0, s1, s2, op0, op1, rd, wr):
        if op1 is None:
            self.add(eng, lambda e: e.tensor_scalar(out=out, in0=in0, scalar1=s1, scalar2=None, op0=op0), rd, wr)
        else:
            self.add(eng, lambda e: e.tensor_scalar(out=out, in0=in0, scalar1=s1, scalar2=s2, op0=op0, op1=op1), rd, wr)

    def tt(self, eng, out, in0, in1, op, rd, wr):
        self.add(eng, lambda e: e.tensor_tensor(out=out, in0=in0, in1=in1, op=op), rd, wr)

    def stt(self, out, in0, scalar, in1, op0, op1, rd, wr):
        self.add('dve', lambda e: e.scalar_tensor_tensor(out=out, in0=in0, scalar=scalar, in1=in1, op0=op0, op1=op1), rd, wr)

    def cp(self, eng, out, in_, rd, wr):
        if eng == 'act':
            self.add('act', lambda e: e.activation(out=out, in_=in_, func=AF.Copy), rd, wr)
        else:
            self.add(eng, lambda e: e.tensor_copy(out=out, in_=in_), rd, wr)

    def recip(self, out, in_, rd, wr):
        self.add('dve', lambda e: e.reciprocal(out=out, in_=in_), rd, wr)

    def dma(self, q, out, in_, rd, wr):
        return self.add(q, lambda e: e.dma_start(out=out, in_=in_), rd, wr, is_dma=True)

    def emit(self, final=False):
        ops = self.ops
        sy = self.sync
        n = len(ops)
        signal = [False] * n
        for o in ops:
            for d in o.deps:
                od = ops[d]
                if od.is_dma:
                    continue
                if od.eng != o.eng or (SAME_SYNC and o.eng != 'pe'):
                    signal[d] = True
        for i, o in enumerate(ops):
            if o.is_dma:
                q = o.eng
                k = sy.dma_rr[q] % NDS
                sy.dma_rr[q] += 1
                o.sem = sy.dma_sems[q][k]
                o.prev = sy.dma_cnt[q][k]
                sy.dma_cnt[q][k] += 16
                o.val = sy.dma_cnt[q][k]
            elif signal[i]:
                sy.eng_cnt[o.eng] += 1
                o.val = sy.eng_cnt[o.eng]
                o.sem = sy.eng_sem[o.eng]
        out_dmas = [o for o in ops if o.is_dma and getattr(o, 'is_out', False)]
        with self.nc.Block() as block:
            for eng in ENGS:
                my = [o for o in ops if o.eng == eng]
                if not my and not (final and eng == 'sp'):
                    continue

                def body(e, eng=eng, my=my):
                    waited = {}

                    def w(sem, val):
                        k = id(sem)
                        if waited.get(k, 0) < val:
                            e.wait_ge(sem, val)
                            waited[k] = val
                    for o in my:
                        if o.is_dma and o.prev > 0:
                            w(o.sem, o.prev)
                        for d in o.deps:
                            od = ops[d]
                            if od.is_dma:
                                w(od.sem, od.val)
                            elif od.eng != eng or (SAME_SYNC and eng != 'pe'):
                                w(od.sem, od.val)
                        ins = o.fn(e)
                        if o.is_dma:
                            ins.then_inc(o.sem, 16)
                        elif o.val is not None:
                            ins.then_inc(o.sem, 1)
                    if eng == 'sp' and final:
                        for q in ('sp', 'pool'):
                            for k in range(NDS):
                                if sy.dma_cnt[q][k] > 0:
                                    w(sy.dma_sems[q][k], sy.dma_cnt[q][k])
                getattr(block, BLOCK_NAME[eng])(body)


class Arena:
    def __init__(self, ap, base):
        self.ap = ap
        self.off = base
        self.base = base

    def f32(self, ncols, shape=None):
        a = self.ap[:, self.off:self.off + ncols]
        self.off += ncols
        assert self.off <= ARENA_COLS, self.off
        return a

    def f32v(self, dims):
        n = int(np.prod(dims))
        a = self.f32(n)
        if len(dims) == 2:
            return a.rearrange("p (a b) -> p a b", a=dims[0])
        return a

    def bf(self, dims):
        n = int(np.prod(dims))
        assert n % 2 == 0
        a = self.f32(n // 2).bitcast(BF16)
        if len(dims) == 2:
            return a.rearrange("p (a b) -> p a b", a=dims[0])
        if len(dims) == 3:
            return a.rearrange("p (a b c) -> p a b c", a=dims[0], b=dims[1])
        return a


C_IDENT, C_RT, C_ONES, C_EPS5, C_EPS6 = 0, 128, 256, 384, 385
C_CSTB = 512
C_MOD = 1024
C_LN = 1216
C_QNG, C_KNG, C_GNG = 1344, 1346, 1348
C_ATTB = 1352
C_KEEP = 1496
C_SCB = 1504
C_CT = 1508
C_BMODT = 1520
PERSIST = 16384 + 2048


class Builder:
    def __init__(self, depth=DEPTH, mixers=True, ne_run=NE, moe=True, layers=None, dbg_stop=99, dbg_sub=99):
        self.dbg_stop = dbg_stop
        self.dbg_sub = dbg_sub
        self.layers = list(range(depth)) if layers is None else layers
        self.depth = depth
        self.mixers = mixers
        self.ne_run = ne_run
        self.moe = moe

    def dram(self, name, shape, dt=F32, out=False):
        return self.nc.dram_tensor(name, list(shape), dt, kind="ExternalOutput" if out else "ExternalInput").ap()

    def build(self):
        from contextlib import ExitStack
        nc = bass.Bass("TRN2", target_bir_lowering=False)
        self.nc = nc
        d = self.dram
        self.x = d("x", [T, D])
        self.cT = d("cT", [128, 8])
        self.st0 = d("st0", [2, 2, 4, 64, 128])
        self.ck = d("ck", [2, 256, 256])
        self.cv = d("cv", [2, 256, 256])
        self.ropeC = d("ropeC", [128, T])
        self.ropeS = d("ropeS", [128, T])
        self.attb = d("attb", [128, 144])
        self.keep = d("keep", [128, 8])
        self.dftC = d("dftC", [T, T], BF16)
        self.dftS = d("dftS", [T, T], BF16)
        self.cs128 = d("cs128", [128, 256], BF16)
        self.cst = d("cst", [128, 512])
        self.cstb = d("cstb", [128, 1024], BF16)
        self.w_mod = d("w_mod", [4, D, 6 * D])
        self.b_modT = d("b_modT", [128, 192])
        self.lnT = d("lnT", [128, 128])
        self.w_in = d("w_in", [2, D, 2176])
        self.w_a2 = d("w_a2", [2, 2, 16, 256])
        self.b_a = d("b_a", [2, 2, 256])
        self.gng = d("gng", [128, 2])
        self.w_oe = d("w_oe", [2, D, D])
        self.w_qkv = d("w_qkv", [2, D, 1536])
        self.qng = d("qng", [128, 2])
        self.kng = d("kng", [128, 2])
        self.kng_bc = d("kng_bc", [128, 256])
        self.w_o = d("w_o", [2, D, D])
        self.w_r = d("w_r", [4, D, NE])
        self.b_r = d("b_r", [4, NE])
        self.w_gu = d("w_gu", [4, NE, D, 2 * D] if self.moe else [1, 1, 128, 128])
        self.b_guT = d("b_guT", [128, 4 * NE * 16])
        self.w_d = d("w_d", [4, NE, D, D] if self.moe else [1, 1, 128, 128])
        self.b_d = d("b_d", [4, NE, D])
        self.y = d("y", [T, D], out=True)
        self.ns = d("ns", [2, 2, 8, 4, 64, 128], out=True)
        self.nk = d("nk", [2, T, 256], out=True)
        self.nv = d("nv", [2, T, 256], out=True)

        with ExitStack() as stack:
            arena = stack.enter_context(nc.sbuf_tensor("arena", [128, ARENA_COLS], F32))
            self.arena = arena[:, :]
            self.ps = [stack.enter_context(nc.psum_tensor("ps%d" % i, [128, 512], F32)) for i in range(8)]
            self.sync = Sync(nc, stack)
            A = self.arena
            self.xT = A[:, 0:16384].rearrange("p (a b) -> p a b", a=8)
            P0 = 16384
            self.cc = lambda off, n: A[:, P0 + off:P0 + off + n]
            self.ident = self.cc(C_IDENT, 128)
            self.RT = self.cc(C_RT, 128)
            self.ones = self.cc(C_ONES, 128)
            self.eps5 = self.cc(C_EPS5, 1)
            self.eps6 = self.cc(C_EPS6, 1)
            cb = self.cc(C_CSTB, 512).bitcast(BF16)
            self.cb = lambda i: cb[:, i * 128:(i + 1) * 128]
            self.mod = self.cc(C_MOD, 192)
            self.lnp = self.cc(C_LN, 128)
            self.attb_sb = self.cc(C_ATTB, 144)
            self.keep_sb = self.cc(C_KEEP, 8)

            self.prologue()
            for l in self.layers:
                if self.mixers:
                    if l % 2 == 0:
                        self.even_layer(l)
                    else:
                        self.odd_layer(l)
                else:
                    self.nomix_layer(l)
                for half in range(2):
                    if self.moe:
                        self.moe_router(l, half)
                        self.moe_experts(l, half)
            self.epilogue()
            self.counts = dict(self.sync.eng_cnt)
        return nc

    def newblk(self):
        return Blk(self.nc, self.sync, self.ps)

    def prologue(self):
        b = self.newblk()
        ps = self.ps
        A = self.arena
        P0 = 16384
        b.dma('sp', self.cc(0, 512), self.cst[:, :], (), ['cst'])
        b.dma('sp', self.cc(C_CSTB, 512).bitcast(BF16), self.cstb[:, :], (), ['cstb'])
        b.dma('sp', self.lnp, self.lnT[:, :], (), ['lnp'])
        b.dma('sp', self.cc(C_QNG, 2), self.qng[:, :], (), ['small'])
        b.dma('sp', self.cc(C_KNG, 2), self.kng[:, :], (), ['small'])
        b.dma('sp', self.cc(C_GNG, 2), self.gng[:, :], (), ['small'])
        b.dma('sp', self.attb_sb, self.attb[:, :], (), ['small'])
        b.dma('sp', self.keep_sb, self.keep[:, :], (), ['small'])
        b.dma('sp', self.cc(C_CT, 8), self.cT[:, :], (), ['cT'])
        b.dma('sp', self.cc(C_BMODT, 192), self.b_modT[:, :], (), ['bmodT'])
        ar = Arena(A, PERSIST)
        xs = [ar.f32(1024) for _ in range(2)]
        for tb in range(NB):
            s = xs[tb % 2]
            b.dma('sp', s, self.x[tb * 128:(tb + 1) * 128, :], (), [('xs', tb % 2)])
            for half in range(2):
                bk = b.bank()
                for c in range(4):
                    kc = half * 4 + c
                    b.tr(ps[bk][:, c * 128:(c + 1) * 128], s[:, kc * 128:(kc + 1) * 128], self.ident,
                         [('xs', tb % 2), 'cst'], [('ps', bk)])
                b.cp('act' if half == 0 else 'dve',
                     self.xT[:, half * 4:half * 4 + 4, tb * 128:(tb + 1) * 128],
                     ps[bk][:, :].rearrange("p (a b) -> p a b", a=4),
                     [('ps', bk)], [('xT', kc, tb // 4) for kc in range(half * 4, half * 4 + 4)])
        scb = self.cc(C_SCB, 4).bitcast(BF16)
        b.act(scb, self.cc(C_CT, 8), AF.Silu, ['cT'], ['scb'])
        wm = [ar.bf([8, 3072]) for _ in range(2)]
        bkM = b.bank()
        it = 0
        for l in range(self.depth):
            for half in range(2):
                w = wm[it % 2]
                for kc in range(KC):
                    b.dma('pool', w[:, kc, :], self.w_mod[l, kc * 128:(kc + 1) * 128, half * 3072:(half + 1) * 3072],
                          (), [('wm', it % 2, kc)])
                for oc in range(24):
                    col = half * 24 + oc
                    for kc in range(KC):
                        b.mm(ps[bkM][:, col:col + 1], w[:, kc, oc * 128:(oc + 1) * 128], scb[:, kc:kc + 1],
                             kc == 0, kc == KC - 1, [('wm', it % 2, kc), 'scb'], [('ps', bkM)])
                it += 1
            b.tt('dve', self.mod[:, l * 48:(l + 1) * 48], ps[bkM][:, 0:48], self.cc(C_BMODT + l * 48, 48), ALU.add,
                 [('ps', bkM), 'bmodT'], [('mod', l)])
            for s in (1, 4):
                sl = self.mod[:, l * 48 + s * 8:l * 48 + s * 8 + 8]
                b.ts('dve', sl, sl, 1.0, None, ALU.add, None, [('mod', l)], [('mod', l)])
        b.emit()

    def ln(self, b, ar, tiles, scale_of, shift_of, eps, out_of, out_keys_of, extra=None, prescale=None,
           src_keys=None):
        ps = self.ps
        sqb = [ar.f32(TT) for _ in range(2)]
        tmp = [ar.f32(TT) for _ in range(4)]
        mean = ar.f32(TT)
        msq = ar.f32(TT)
        rstd = ar.f32(TT)
        nmr = ar.f32(TT)
        for t in tiles:
            sl = slice(t * TT, (t + 1) * TT)
            bm = b.bank()
            bq = b.bank()
            for kc in range(KC):
                xk = ('xT', kc, t)
                sq = sqb[kc % 2]
                b.act(sq, self.xT[:, kc, sl], AF.Square, [xk], [('sqb', kc % 2)])
                b.mm(ps[bm][:, :], self.ones, self.xT[:, kc, sl], kc == 0, kc == KC - 1, [xk, 'cst'], [('ps', bm)])
                b.mm(ps[bq][:, :], self.ones, sq, kc == 0, kc == KC - 1, [('sqb', kc % 2), 'cst'], [('ps', bq)])
            b.act(mean, ps[bm][:, :], AF.Copy, [('ps', bm)], ['ln_mean'], scale=1.0 / D)
            b.tt('dve', msq, mean, mean, ALU.mult, ['ln_mean'], ['ln_msq'])
            b.stt(msq, ps[bq][:, :], 1.0 / D, msq, ALU.mult, ALU.subtract, [('ps', bq), 'ln_msq'], ['ln_msq'])
            b.act(msq, msq, AF.Sqrt, ['ln_msq', 'cst'], ['ln_msq'], bias=eps)
            b.recip(rstd, msq, ['ln_msq'], ['ln_rstd'])
            b.stt(nmr, mean, -1.0, rstd, ALU.mult, ALU.mult, ['ln_mean', 'ln_rstd'], ['ln_nmr'])
            for kc in range(KC):
                xk = ('xT', kc, t)
                tp = tmp[kc % 4]
                tk = ('lntmp', kc % 4)
                b.tt('dve', tp, self.xT[:, kc, sl], rstd, ALU.mult, [xk, 'ln_rstd'], [tk])
                b.tt('dve', tp, tp, nmr, ALU.add, [tk, 'ln_nmr'], [tk])
                b.act(out_of(kc, t), tp, AF.Identity, [tk, 'mods'], out_keys_of(kc, t),
                      bias=shift_of(kc), scale=scale_of(kc))
                if extra is not None:
                    eo, ek = extra(kc, t)
                    b.act(eo, tp, AF.Identity, [tk, 'mods'], ek, bias=shift_of(kc), scale=scale_of(kc))
                if prescale is not None:
                    b.ts('pool', self.xT[:, kc, sl], self.xT[:, kc, sl], float(prescale), None, ALU.mult, None,
                         [xk], [xk])

    def modcol(self, l, s, kc):
        i = l * 48 + s * 8 + kc
        return self.mod[:, i:i + 1]

    def lncol(self, l, which, gb, kc):
        i = ((l * 2 + which) * 2 + gb) * 8 + kc
        return self.lnp[:, i:i + 1]

    def nomix_layer(self, l):
        b = self.newblk()
        ar = Arena(self.arena, PERSIST)
        for kc in range(KC):
            for t in range(NT):
                sl = slice(t * TT, (t + 1) * TT)
                b.ts('pool', self.xT[:, kc, sl], self.xT[:, kc, sl], float(ALPHA), None, ALU.mult, None,
                     [('xT', kc, t)], [('xT', kc, t)])
        self.ln(b, ar, range(NT), lambda kc: self.lncol(l, 0, 0, kc), lambda kc: self.lncol(l, 0, 1, kc), self.eps5,
                lambda kc, t: self.xT[:, kc, t * TT:(t + 1) * TT], lambda kc, t: [('xT', kc, t)])
        b.emit()

    def moe_router(self, l, half):
        b = self.newblk()
        ps = self.ps
        ar = Arena(self.arena, PERSIST)
        self.hT = ar.bf([8, 1024])
        self.combT = ar.bf([1024])
        h32 = ar.f32v([8, TT])
        wr = ar.f32v([8, NE])
        br = ar.f32(NE)
        lg = ar.f32(NE)
        m8 = ar.f32(8)
        nmax = ar.f32(1)
        ex = ar.f32(NE)
        mask = ar.f32(NE)
        ssum = ar.f32(1)
        comb = ar.f32(NE)
        b.dma('sp', wr, self.w_r[l].rearrange("(kc p) n -> p kc n", p=128), (), ['wr'])
        b.dma('sp', br[0:1, :], self.b_r[l:l + 1, :], (), ['br'])
        tiles = [half * 2, half * 2 + 1]

        def out_of(kc, t):
            tl = t - half * 2
            return self.hT[:, kc, tl * TT:(tl + 1) * TT]

        lnar = Arena(self.arena, ar.off)
        first = True
        for t in tiles:
            tl = t - half * 2
            sub = Arena(self.arena, lnar.base)
            self.ln(b, sub, [t], lambda kc: self.modcol(l, 4, kc), lambda kc: self.modcol(l, 3, kc), self.eps5,
                    out_of, lambda kc, t: [('hT', kc, t - half * 2)],
                    extra=lambda kc, t: (h32[:, kc, :], [('h32', kc)]), prescale=ALPHA)
            for q in range(4):
                tbl = tl * 4 + q
                bk = b.bank()
                b.mm(ps[bk][:, 0:NE], self.ones[0:1, 0:128], br[0:1, :], True, False, ['cst', 'br'], [('ps', bk)])
                for kc in range(KC):
                    b.mm(ps[bk][:, 0:NE], h32[:, kc, q * 128:(q + 1) * 128], wr[:, kc, :], False, kc == KC - 1,
                         [('h32', kc), 'wr'], [('ps', bk)])
                b.cp('act', lg, ps[bk][:, 0:NE], [('ps', bk)], ['lg'])
                b.add('dve', lambda e: e.max(out=m8, in_=lg), ['lg'], ['m8'])
                b.ts('dve', nmax, m8[:, 0:1], -1.0, None, ALU.mult, None, ['m8'], ['nmax'])
                b.act(ex, lg, AF.Exp, ['lg', 'nmax'], ['ex'], bias=nmax)
                b.ts('dve', mask, lg, m8[:, 3:4], None, ALU.is_ge, None, ['lg', 'm8'], ['mask'])
                b.tt('dve', ex, ex, mask, ALU.mult, ['ex', 'mask'], ['ex'])
                b.add('dve', lambda e: e.reduce_sum(out=ssum, in_=ex, axis=AX.X), ['ex'], ['ssum'])
                b.recip(ssum, ssum, ['ssum'], ['ssum'])
                b.ts('dve', comb, ex, ssum, None, ALU.mult, None, ['ex', 'ssum'], ['comb'])
                bk2 = b.bank()
                b.tr(ps[bk2][0:NE, 0:128], comb, self.ident, ['comb', 'cst'], [('ps', bk2)])
                b.cp('act', self.combT[0:NE, tbl * 128:(tbl + 1) * 128], ps[bk2][0:NE, 0:128], [('ps', bk2)],
                     [('combT', tbl)])
        self.moe_off = ar.off - 0
        b.emit()

    def moe_experts(self, l, half):
        b = self.newblk()
        ps = self.ps
        ar = Arena(self.arena, PERSIST)
        hT = ar.bf([8, 1024])
        combT = ar.bf([1024])
        actT = ar.bf([8, 1024])
        WGa = [ar.bf([4, 2048]) for _ in range(2)]
        WGb = ar.bf([4, 2048])
        WD = ar.bf([8, 1024])
        comb_b = ar.bf([1024])
        cmask = ar.bf([1024])
        gb = [ar.f32(TT) for _ in range(3)]
        sb = [ar.f32(TT) for _ in range(3)]
        ub = [ar.f32(TT) for _ in range(3)]
        bgu = ar.f32(512)
        bd = ar.bf([1024])
        ident = self.ident
        onesb = self.cb(7)
        b.dma('sp', bgu, self.b_guT[:, l * 512:(l + 1) * 512], (), ['bgu'])
        b.dma('pool', bd[0:NE, :], self.b_d[l], (), ['bd'])
        bgu3 = bgu.rearrange("p (e c) -> p e c", e=NE)
        b.ts('dve', bgu3[:, :, 8:16], bgu3[:, :, 8:16], 1.0, None, ALU.add, None, ['bgu'], ['bgu'])
        g2 = lambda m: self.modcol(l, 5, m)

        def wgu(e, kc):
            return WGa[e % 2][:, kc, :] if kc < 4 else WGb[:, kc - 4, :]

        def wkey(e, kc):
            return ('wgu', kc, e % 2) if kc < 4 else ('wgu', kc)

        def load_gu_a(e):
            for kc in range(4):
                b.dma('pool', wgu(e, kc), self.w_gu[l, e, kc * 128:(kc + 1) * 128, :], (), [wkey(e, kc)])

        def load_gu_b(e):
            for kc in range(4, KC):
                b.dma('pool', wgu(e, kc), self.w_gu[l, e, kc * 128:(kc + 1) * 128, :], (), [wkey(e, kc)])

        def load_d(e):
            for jc in range(KC):
                b.dma('pool', WD[:, jc, :], self.w_d[l, e, jc * 128:(jc + 1) * 128, :], (), [('wd', jc)])

        def comb_prep(e):
            b.ts('dve', cmask[0:NE, :], combT[0:NE, :], ident[0:NE, e:e + 1], None, ALU.mult, None,
                 [('combT', i) for i in range(8)] + ['cst'], ['cmask'])
            for t in range(2):
                bk = b.bank()
                b.mm(ps[bk][:, :], onesb[0:NE, :], cmask[0:NE, t * TT:(t + 1) * TT], True, True,
                     ['cmask', 'cstb'], [('ps', bk)])
                b.cp('act', comb_b[:, t * TT:(t + 1) * TT], ps[bk][:, :], [('ps', bk)], [('comb_b', t)])

        load_gu_a(0)
        load_gu_b(0)
        load_d(0)
        comb_prep(0)
        it = 0
        pend = None
        NEr = self.ne_run
        for e in range(NEr):
            if e + 1 < NEr:
                load_gu_a(e + 1)
            for j in range(KC):
                for t in range(2):
                    sl = slice(t * TT, (t + 1) * TT)
                    bg = b.bank()
                    bu = b.bank()
                    for kc in range(KC):
                        b.mm(ps[bg][:, :], wgu(e, kc)[:, j * 128:(j + 1) * 128], hT[:, kc, sl], kc == 0, kc == KC - 1,
                             [wkey(e, kc), ('hT', kc, t)], [('ps', bg)])
                    for kc in range(KC):
                        b.mm(ps[bu][:, :], wgu(e, kc)[:, D + j * 128:D + (j + 1) * 128], hT[:, kc, sl], kc == 0,
                             kc == KC - 1, [wkey(e, kc), ('hT', kc, t)], [('ps', bu)])
                    i = it % 3
                    it += 1
                    G, S, U = gb[i], sb[i], ub[i]
                    cg = e * 16 + j
                    cu = e * 16 + 8 + j
                    b.ts('dve', G, ps[bg][:, :], bgu[:, cg:cg + 1], 7.0, ALU.add, ALU.min, [('ps', bg), 'bgu'], [('G', i)])
                    b.ts('dve', U, ps[bu][:, :], bgu[:, cu:cu + 1], 8.0, ALU.add, ALU.min, [('ps', bu), 'bgu'], [('U', i)])
                    b.act(S, G, AF.Sigmoid, [('G', i)], [('S', i)], scale=1.702)
                    b.tt('pool', S, G, S, ALU.mult, [('G', i), ('S', i)], [('S', i)])
                    if pend is not None:
                        pend()

                    def tail(S=S, U=U, i=i, j=j, sl=sl, t=t):
                        b.stt(U, U, -6.0, S, ALU.max, ALU.mult, [('U', i), ('S', i)], [('U', i)])
                        b.tt('pool', actT[:, j, sl], U, comb_b[:, sl], ALU.mult, [('U', i), ('comb_b', t)], [('actT', j, t)])
                    pend = tail
            pend()
            pend = None
            if e + 1 < NEr:
                load_gu_b(e + 1)
                comb_prep(e + 1)
            for t in range(2):
                for m in range(KC):
                    sl = slice(t * TT, (t + 1) * TT)
                    gsl = slice((half * 2 + t) * TT, (half * 2 + t + 1) * TT)
                    bk = b.bank()
                    if e == 0:
                        b.mm(ps[bk][:, :], bd[0:NE, m * 128:(m + 1) * 128], combT[0:NE, sl], True, False,
                             ['bd'] + [('combT', t * 4 + q) for q in range(4)], [('ps', bk)])
                    for j in range(KC):
                        b.mm(ps[bk][:, :], WD[:, j, m * 128:(m + 1) * 128], actT[:, j, sl], (j == 0 and e != 0),
                             j == KC - 1, [('wd', j), ('actT', j, t)], [('ps', bk)])
                    xk = ('xT', m, half * 2 + t)
                    b.stt(self.xT[:, m, gsl], ps[bk][:, :], g2(m), self.xT[:, m, gsl], ALU.mult, ALU.add,
                          [('ps', bk), xk, 'mods'], [xk])
            if e + 1 < NEr:
                load_d(e + 1)
        b.emit()
        b = self.newblk()
        ar = Arena(self.arena, PERSIST)
        self.ln(b, ar, [half * 2, half * 2 + 1], lambda kc: self.lncol(l, 1, 0, kc), lambda kc: self.lncol(l, 1, 1, kc),
                self.eps5, lambda kc, t: self.xT[:, kc, t * TT:(t + 1) * TT], lambda kc, t: [('xT', kc, t)])
        b.emit()

    def epilogue(self):
        b = self.newblk()
        ps = self.ps
        ar = Arena(self.arena, PERSIST)
        ys = [ar.f32(512) for _ in range(4)]
        it = 0
        for tb in range(NB):
            for half in range(2):
                bk = b.bank()
                for c in range(4):
                    kc = half * 4 + c
                    b.tr(ps[bk][:, c * 128:(c + 1) * 128], self.xT[:, kc, tb * 128:(tb + 1) * 128], self.ident,
                         [('xT', kc, tb // 4), 'cst'], [('ps', bk)])
                s = ys[it % 4]
                b.cp('act' if it % 2 == 0 else 'dve', s, ps[bk][:, :], [('ps', bk)], [('ys', it % 4)])
                b.dma('sp', self.y[tb * 128:(tb + 1) * 128, half * 512:(half + 1) * 512], s, [('ys', it % 4)], [])
                it += 1
        b.emit(final=True)


    def even_layer(self, l):
        i = l // 2
        A = self.arena
        ps = self.ps
        ident, ones, onesb = self.ident, self.ones, self.cb(7)
        OUTS = self.OUTS

        def region(off, dims):
            a = Arena(A, PERSIST + off)
            return a.bf(dims)

        ufT = region(0, [4, 2048])
        qT = region(4096, [2, 2048])
        kT = region(6144, [2, 2048])
        sgT = region(8192, [4, 2048])
        alrT = region(12288, [2048])
        k_tm = region(13312, [16, 256])
        v_tm = region(15360, [16, 512])
        C_EL = 1712
        elast = self.cc(C_EL, 128)

        self.pre_ln_block(l, OUTS)

        b = self.newblk()
        ar = Arena(A, PERSIST + OUTS)
        hT = ar.bf([8, 2048])
        Wp = [ar.bf([8, 512]) for _ in range(2)]

        def load_piece(p):
            W = Wp[p % 2]
            ncol = 512 if p < 4 else 128
            for kc in range(KC):
                b.dma('pool', W[:, kc, 0:ncol], self.w_in[i, kc * 128:(kc + 1) * 128, p * 512:p * 512 + ncol], (),
                      [('Wp', p % 2, kc)])
            return W

        ev = [0]

        def fm(W, pidx, c0, evac):
            for t in range(NT):
                sl = slice(t * TT, (t + 1) * TT)
                bk = b.bank()
                for kc in range(KC):
                    b.mm(ps[bk][:, :], W[:, kc, c0:c0 + 128], hT[:, kc, sl], kc == 0, kc == KC - 1,
                         [('Wp', pidx % 2, kc), ('hT', kc, t)], [('ps', bk)])
                evac(bk, t, sl)

        def tm(W, pidx, c0, n, dst, dkey):
            for tb in range(NB):
                bk = b.bank()
                for kc in range(KC):
                    b.mm(ps[bk][:, 0:n], hT[:, kc, tb * 128:(tb + 1) * 128], W[:, kc, c0:c0 + n], kc == 0,
                         kc == KC - 1, [('Wp', pidx % 2, kc), ('hT', kc, tb // 4)], [('ps', bk)])
                ev[0] += 1
                b.cp('act' if ev[0] % 2 else 'dve', dst[:, tb, :], ps[bk][:, 0:n], [('ps', bk)], [(dkey, tb)])

        def cp_to(dst3, key):
            def f(bk, t, sl):
                ev[0] += 1
                b.cp('act' if ev[0] % 2 else 'dve', dst3(sl), ps[bk][:, :], [('ps', bk)], [(key, t)])
            return f

        W = load_piece(0)
        W1 = load_piece(1)
        for g in range(4):
            fm(W, 0, g * 128, cp_to(lambda sl, g=g: ufT[:, g, sl], ('ufT', g)))
        W = W1
        for c in range(2):
            def evq(bk, t, sl, c=c):
                b.act(qT[:, c, sl], ps[bk][:, :], AF.Copy, [('ps', bk)], [('qT', c, t)], scale=0.125)
            fm(W, 1, c * 128, evq)
            fm(W, 1, 256 + c * 128, cp_to(lambda sl, c=c: kT[:, c, sl], ('kT', c)))
        tm(W, 1, 256, 256, k_tm, 'k_tm')
        W = load_piece(2)
        tm(W, 2, 0, 512, v_tm, 'v_tm')
        W = load_piece(3)
        for h in range(4):
            def evg(bk, t, sl, h=h):
                b.act(sgT[:, h, sl], ps[bk][:, :], AF.Silu, [('ps', bk)], [('sgT', h, t)])
            fm(W, 3, h * 128, evg)
        W = load_piece(4)
        fm(W, 4, 0, cp_to(lambda sl: alrT[:, sl], 'alrT'))
        b.emit()

        if self.dbg_stop <= 2:
            return self.post_ln_block(l)
        b = self.newblk()
        ar = Arena(A, PERSIST + OUTS)
        AB = ar.bf([16, 4, 256])
        Ctab = ar.bf([16, 256])
        Stab = ar.bf([16, 256])
        cs = ar.bf([256])
        b.dma('sp', cs, self.cs128[:, :], (), ['cs'])
        for tb in range(NB):
            for gp in range(2):
                bk = 4 + (tb * 2 + gp) % 4
                for gg in range(2):
                    g = gp * 2 + gg
                    b.mm(ps[bk][:, gg * 256:(gg + 1) * 256], ufT[:, g, tb * 128:(tb + 1) * 128], cs, True, True,
                         [(('ufT', g), tb // 4), 'cs'], [('ps', bk)])
                b.cp('act' if gp == 0 else 'dve', AB[:, tb, gp * 2:gp * 2 + 2, :],
                     ps[bk][:, :].rearrange("p (a b) -> p a b", a=2), [('ps', bk)], [('AB', tb, gp * 2), ('AB', tb, gp * 2 + 1)])
        dC = self.dftC.rearrange("(tb p) n -> p tb n", p=128)
        dS = self.dftS.rearrange("(tb p) n -> p tb n", p=128)
        for tq in range(8):
            qs = slice(tq * 256, (tq + 1) * 256)
            b.dma('sp', Ctab, dC[:, :, qs], (), ['Ctab'])
            b.dma('sp', Stab, dS[:, :, qs], (), ['Stab'])
            for g in range(4):
                for tb in range(NB):
                    b.mm(ps[g][:, 0:256], AB[:, tb, g, 0:128], Ctab[:, tb, :], tb == 0, False,
                         [('AB', tb, g), 'Ctab'], [('ps', g)])
            for g in range(4):
                for tb in range(NB):
                    b.mm(ps[g][:, 0:256], AB[:, tb, g, 128:256], Stab[:, tb, :], False, tb == NB - 1,
                         [('AB', tb, g), 'Stab'], [('ps', g)])
            for g in range(4):
                b.cp('act' if g % 2 == 0 else 'dve', ufT[:, g, qs], ps[g][:, 0:256], [('ps', g)],
                     [(('ufT', g), tq // 2)])
        b.emit()

        if self.dbg_stop <= 3:
            return self.post_ln_block(l)
        b = self.newblk()
        ar = Arena(A, PERSIST + OUTS)
        q0 = ar.bf([2, 2048])
        k0 = ar.bf([2, 2048])
        kh0 = ar.bf([16, 256])
        logg = ar.bf([16, 512])
        wa = ar.bf([512])
        ba = ar.bf([512])
        tmpa = [ar.f32(512) for _ in range(2)]
        tmpb = [ar.f32(512) for _ in range(2)]
        qd = [q0, qT]
        kd = [k0, kT]
        khd = [kh0, k_tm]
        b.add('dve', lambda e: e.memset(wa, 0.0), (), ['wa'])
        b.add('dve', lambda e: e.memset(ba, 0.0), (), ['ba'])
        b.dma('pool', wa[0:16, 0:256], self.w_a2[i, 0], (), ['wa'])
        b.dma('pool', wa[32:48, 256:512], self.w_a2[i, 1], (), ['wa'])
        b.dma('pool', ba[0:1, :], self.b_a[i].rearrange("(o z) e -> o (z e)", o=1), (), ['ba'])
        one_col = self.ones[:, 0:1]
        E0 = self.cb(0)
        for tb in range(NB):
            bk = b.bank()
            tsl = slice(tb * 128, (tb + 1) * 128)
            b.mm(ps[bk][:, :], E0, ba, True, False, ['cstb', 'ba'], [('ps', bk)])
            b.mm(ps[bk][:, :], alrT[:, tsl], wa, False, True, ['wa'], [('ps', bk)])
            tp = tmpa[tb % 2]
            tk = ('tmpa', tb % 2)
            b.act(tp, ps[bk][:, :], AF.Exp, [('ps', bk)], [tk], scale=-1.0)
            b.act(tp, tp, AF.Ln, [tk], [tk], bias=one_col)
            b.ts('dve', logg[:, tb, :], tp, -1.0 / 16.0, None, ALU.mult, None, [tk], [('logg', tb)])
        itc = 0
        for d in range(2 if self.dbg_sub >= 2 else 0):
            tri = self.cb(1 + d)
            for c in range(2):
                for t in range(NT):
                    sl = slice(t * TT, (t + 1) * TT)
                    bk = b.bank()
                    for q in range(4):
                        tb = t * 4 + q
                        b.mm(ps[bk][:, q * 128:(q + 1) * 128], logg[:, tb, d * 256 + c * 128:d * 256 + (c + 1) * 128],
                             tri, True, True, [('logg', tb), 'cstb'], [('ps', bk)])
                    pos = 63 if d == 0 else 0
                    e0 = (d * 2 + c) * 32 + t * 8
                    b.act(elast[:, e0:e0 + 8], ps[bk][:, :].rearrange("p (a b) -> p a b", a=8)[:, :, pos], AF.Exp,
                          [('ps', bk)], [('elast', d, c, t)])
                    ta = tmpa[itc % 2]
                    tbq = tmpb[itc % 2]
                    ka, kb = ('tmpa', itc % 2), ('tmpb', itc % 2)
                    itc += 1
                    b.act(ta, ps[bk][:, :], AF.Exp, [('ps', bk)], [ka])
                    b.act(tbq, ps[bk][:, :], AF.Exp, [('ps', bk)], [kb], scale=-1.0)
                    b.tt('dve', qd[d][:, c, sl], qT[:, c, sl], ta, ALU.mult, [('qT', c, t), ka], [('qd', d, c, t), ('qT', c, t)] if d == 1 else [('qd', d, c, t)])
                    b.tt('pool', kd[d][:, c, sl], kT[:, c, sl], tbq, ALU.mult, [('kT', c, t), kb], [('kd', d, c, t), ('kT', c, t)] if d == 1 else [('kd', d, c, t)])
            sm = self.cb(3 + d)
            for tb2 in range(NB // 2 if self.dbg_sub >= 3 else 0):
                bk = b.bank()
                for q in range(2):
                    tb = tb2 * 2 + q
                    b.mm(ps[bk][:, q * 256:(q + 1) * 256], sm, logg[:, tb, d * 256:(d + 1) * 256], True, True,
                         [('logg', tb), 'cstb'], [('ps', bk)])
                ta = tmpa[itc % 2]
                ka = ('tmpa', itc % 2)
                itc += 1
                b.act(ta, ps[bk][:, :], AF.Exp, [('ps', bk)], [ka])
                b.tt('dve', khd[d][:, tb2 * 2:tb2 * 2 + 2, :], k_tm[:, tb2 * 2:tb2 * 2 + 2, :],
                     ta.rearrange("p (a b) -> p a b", a=2), ALU.mult, [('k_tm', tb2), ka],
                     [('khd', d, tb2), ('k_tm', tb2)] if d == 1 else [('khd', d, tb2)])
        b.emit()

        if self.dbg_stop <= 4:
            return self.post_ln_block(l)
        b = self.newblk()
        Sbf = Arena(A, PERSIST + 25600).bf([128, 128])
        Mst = Arena(A, PERSIST + 12288).f32v([4, 256])
        for s4 in range(4):
            b.add('dve', lambda e, s4=s4: e.memset(Mst[:, s4, :], 0.0), (), [('M', s4)])
        for d in range(2):
            for p in range(2):
                s4 = d * 2 + p
                b.dma('sp', Mst[0:64, s4, 0:128], self.st0[i, d, 2 * p], (), [('M', s4)])
                b.dma('sp', Mst[64:128, s4, 128:256], self.st0[i, d, 2 * p + 1], (), [('M', s4)])
        for step in range(32):
            for d in range(2):
                c = step if d == 0 else 31 - step
                tb = c // 2
                r0 = (c % 2) * 64
                for p in range(2):
                    s4 = d * 2 + p
                    M = Mst[:, s4, :]
                    sidx = s4 * 32 + c
                    b.cp('act', Sbf[0:64, sidx, :], M[0:64, 0:128], [('M', s4)], [('Sbf', sidx)])
                    b.cp('pool', Sbf[64:128, sidx, :], M[64:128, 128:256], [('M', s4)], [('Sbf', sidx)])
                    bk = b.bank()
                    b.mm(ps[bk][:, 0:256], khd[d][r0:r0 + 64, tb, p * 128:(p + 1) * 128],
                         v_tm[r0:r0 + 64, tb, p * 256:(p + 1) * 256], True, True, [], [('ps', bk)])
                    ei = (d * 2 + p) * 32 + c
                    b.stt(M, M, elast[:, ei:ei + 1], ps[bk][:, 0:256], ALU.mult, ALU.add, [('M', s4), ('ps', bk)],
                          [('M', s4)])
                    boundary = (c % 4 == 3) if d == 0 else (c % 4 == 0)
                    if boundary:
                        n = c // 4
                        b.dma('sp', self.ns[i, d, n, 2 * p], M[0:64, 0:128], [('M', s4)], [])
                        b.dma('sp', self.ns[i, d, n, 2 * p + 1], M[64:128, 128:256], [('M', s4)], [])
                        b.ts('dve', M, M, self.keep_sb[:, n:n + 1], None, ALU.mult, None, [('M', s4)], [('M', s4)])
        b.emit()

        if self.dbg_stop <= 5:
            return self.post_ln_block(l)
        b = self.newblk()
        atm = Arena(A, PERSIST + 12288)
        attm = [atm.bf([256]) for _ in range(2)]
        sqb = atm.f32(256)
        isb = atm.f32(256)
        oT = [Arena(A, PERSIST + 13312).bf([2, 2048]), Arena(A, PERSIST + 23552).bf([2, 2048])]
        bm2 = self.cc(C_CSTB, 512).bitcast(BF16)[:, 5 * 128:7 * 128]
        gcol = self.cc(C_GNG + i, 1)
        ita = 0
        for h in range(4):
            p = h // 2
            r0 = (h % 2) * 64
            for t2 in range(8):
                sl = slice(t2 * 256, (t2 + 1) * 256)
                bo = t2 % 2
                bi = 2 + t2 % 2
                for q in range(2):
                    tb = t2 * 2 + q
                    tsl = slice(tb * 128, (tb + 1) * 128)
                    ba_ = 4 + ita % 2
                    am = attm[ita % 2]
                    ak = ('attm', ita % 2)
                    ita += 1
                    for d in range(2):
                        b.mm(ps[ba_][:, d * 128:(d + 1) * 128], kd[d][r0:r0 + 64, p, tsl], qd[d][r0:r0 + 64, p, tsl],
                             True, True, [], [('ps', ba_)])
                    b.tt('dve', am, ps[ba_][:, 0:256], bm2, ALU.mult, [('ps', ba_)], [ak])
                    oc = ps[bo][:, q * 128:(q + 1) * 128]
                    b.mm(oc, v_tm[:, tb, h * 128:(h + 1) * 128], am[:, 0:128], True, False, [ak], [('ps', bo)])
                    b.mm(oc, v_tm[:, tb, h * 128:(h + 1) * 128], am[:, 128:256], False, True, [ak], [('ps', bo)])
                    for cc in range(2):
                        c = tb * 2 + cc
                        for d in range(2):
                            sidx = (d * 2 + p) * 32 + c
                            b.mm(ps[bi][:, q * 128 + cc * 64:q * 128 + (cc + 1) * 64], Sbf[r0:r0 + 64, sidx, :],
                                 qd[d][r0:r0 + 64, p, c * 64:(c + 1) * 64], d == 0, d == 1, [], [('ps', bi)])
                b.cp('act', isb, ps[bi][:, 0:256], [('ps', bi)], ['isb'])
                b.tt('dve', isb, ps[bo][:, 0:256], isb, ALU.add, [('ps', bo), 'isb'], ['isb'])
                b.act(sqb, isb, AF.Square, ['isb'], ['sqb'])
                b.mm(ps[6][:, 0:256], ones, sqb, True, True, ['sqb'], [('ps', 6)])
                b.act(sqb, ps[6][:, 0:256], AF.Sqrt, [('ps', 6)], ['sqb'], bias=self.eps6, scale=1.0 / 128)
                b.recip(sqb, sqb, ['sqb'], ['sqb'])
                b.tt('dve', isb, isb, sqb, ALU.mult, ['isb', 'sqb'], ['isb'])
                b.stt(oT[p][:, h % 2, sl], isb, gcol, sgT[:, h, sl], ALU.mult, ALU.mult, ['isb'], [('oT', h, t2)])
        b.emit()

        if self.dbg_stop <= 6:
            return self.post_ln_block(l)
        b = self.newblk()
        Wo = Arena(A, PERSIST + 25600).bf([8, 1024])
        for kc in range(KC):
            b.dma('pool', Wo[:, kc, :], self.w_oe[i, kc * 128:(kc + 1) * 128, :], (), [('Wo', kc)])
        for m in range(KC):
            for t in range(NT):
                sl = slice(t * TT, (t + 1) * TT)
                bk = b.bank()
                for kk in range(8):
                    rhs = ufT[:, kk, sl] if kk < 4 else oT[(kk - 4) // 2][:, (kk - 4) % 2, sl]
                    b.mm(ps[bk][:, :], Wo[:, kk, m * 128:(m + 1) * 128], rhs, kk == 0, kk == 7, [('Wo', kk)], [('ps', bk)])
                xk = ('xT', m, t)
                b.stt(self.xT[:, m, sl], ps[bk][:, :], self.modcol(l, 2, m), self.xT[:, m, sl], ALU.mult, ALU.add,
                      [('ps', bk), xk], [xk])
        b.emit()
        self.post_ln_block(l)


    OUTS = 19456

    def pre_ln_block(self, l, base):
        b = self.newblk()
        ar = Arena(self.arena, PERSIST + base)
        hT = ar.bf([8, 2048])
        self.ln(b, ar, range(NT), lambda kc: self.modcol(l, 1, kc), lambda kc: self.modcol(l, 0, kc), self.eps5,
                lambda kc, t: hT[:, kc, t * TT:(t + 1) * TT], lambda kc, t: [('hT', kc, t)], prescale=ALPHA)
        b.emit()

    def post_ln_block(self, l):
        b = self.newblk()
        ar = Arena(self.arena, PERSIST)
        self.ln(b, ar, range(NT), lambda kc: self.lncol(l, 0, 0, kc), lambda kc: self.lncol(l, 0, 1, kc), self.eps5,
                lambda kc, t: self.xT[:, kc, t * TT:(t + 1) * TT], lambda kc, t: [('xT', kc, t)])
        b.emit()

    def odd_layer(self, l):
        i = l // 2
        self.pre_ln_block(l, 0)
        b = self.newblk()
        ps = self.ps
        ar = Arena(self.arena, PERSIST)
        hT = ar.bf([8, 2048])
        qT = ar.bf([8, 2048])
        W = ar.bf([8, 1536])
        kT = ar.bf([2, 2304])
        vS = ar.bf([18, 256])
        rC = ar.f32(512)
        rS = ar.f32(512)
        y32 = ar.f32(512)
        sq32 = ar.f32(512)
        rs = ar.f32(512)
        t1 = ar.f32(512)
        pT = [ar.bf([512]) for _ in range(3)]
        rden = ar.f32(512)
        v32 = [ar.f32(256) for _ in range(2)]
        k32 = [ar.f32(256) for _ in range(2)]
        cks = ar.f32v([2, 256])
        kbc = ar.f32(128)
        ssq = ar.f32(2)
        ident, ones, onesb = self.ident, self.ones, self.cb(7)
        qg = self.cc(C_QNG + i, 1)
        kg = self.cc(C_KNG + i, 1)
        for kc in range(KC):
            b.dma('pool', W[:, kc, :], self.w_qkv[i, kc * 128:(kc + 1) * 128, :], (), [('W', kc)])
        b.dma('sp', cks, self.ck[i].rearrange("(sb p) f -> p sb f", p=128), (), ['cks'])
        b.dma('pool', vS[:, 0:2, :], self.cv[i].rearrange("(sb p) f -> p sb f", p=128), (), [('vS', 0), ('vS', 1)])
        b.dma('sp', kbc, self.kng_bc[:, i * 128:(i + 1) * 128], (), ['kbc'])
        for sbi in range(2):
            bk = 7
            for kv in range(2):
                b.tr(ps[bk][:, kv * 128:(kv + 1) * 128], cks[:, sbi, kv * 128:(kv + 1) * 128], ident,
                     ['cks', 'cst'], [('ps', bk)])
            for kv in range(2):
                b.cp('act', kT[:, kv, sbi * 128:(sbi + 1) * 128], ps[bk][:, kv * 128:(kv + 1) * 128],
                     [('ps', bk)], [('kT', kv, sbi)])
        for tb in range(NB):
            bk = 4 + tb % 4
            tsl = slice(tb * 128, (tb + 1) * 128)
            for kc in range(KC):
                b.mm(ps[bk][:, 0:256], hT[:, kc, tsl], W[:, kc, 1280:1536], kc == 0, kc == KC - 1,
                     [('hT', kc, tb // 4), ('W', kc)], [('ps', bk)])
            for kc in range(KC):
                b.mm(ps[bk][:, 256:512], hT[:, kc, tsl], W[:, kc, 1024:1280], kc == 0, kc == KC - 1,
                     [('hT', kc, tb // 4), ('W', kc)], [('ps', bk)])
            v = v32[tb % 2]
            k = k32[tb % 2]
            b.cp('act', v, ps[bk][:, 0:256], [('ps', bk)], [('v32', tb % 2)])
            b.dma('sp', self.nv[i, tsl, :], v, [('v32', tb % 2)], [])
            b.cp('pool', vS[:, 2 + tb, :], v, [('v32', tb % 2)], [('vS', 2 + tb)])
            for kv in range(2):
                b.act(k[:, kv * 128:(kv + 1) * 128], ps[bk][:, 256 + kv * 128:256 + (kv + 1) * 128], AF.Square,
                      [('ps', bk)], [('k32', tb % 2), ('ssq', kv)], accum_out=ssq[:, kv:kv + 1])
            b.act(ssq, ssq, AF.Sqrt, [('ssq', 0), ('ssq', 1), 'cst'], [('ssq', 0), ('ssq', 1)], bias=self.eps6,
                  scale=1.0 / 128)
            b.recip(ssq, ssq, [('ssq', 0), ('ssq', 1)], [('ssq', 0), ('ssq', 1)])
            for kv in range(2):
                b.stt(k[:, kv * 128:(kv + 1) * 128], ps[bk][:, 256 + kv * 128:256 + (kv + 1) * 128],
                      ssq[:, kv:kv + 1], kbc, ALU.mult, ALU.mult, [('ps', bk), ('ssq', kv), 'kbc'], [('k32', tb % 2)])
            b.dma('sp', self.nk[i, tsl, :], k, [('k32', tb % 2)], [])
        for t in range(NT):
            sl = slice(t * TT, (t + 1) * TT)
            b.dma('sp', rC, self.ropeC[:, sl], (), ['rC'])
            b.dma('sp', rS, self.ropeS[:, sl], (), ['rS'])
            for hc in range(10):
                col0 = hc * 128 if hc < 8 else 1024 + (hc - 8) * 128
                gcol = qg if hc < 8 else kg
                bq = 4 + hc % 2
                for kc in range(KC):
                    b.mm(ps[bq][:, :], W[:, kc, col0:col0 + 128], hT[:, kc, sl], kc == 0, kc == KC - 1,
                         [('W', kc), ('hT', kc, t)], [('ps', bq)])
                b.act(y32, ps[bq][:, :], AF.Identity, [('ps', bq), 'small'], ['y32'], scale=gcol)
                b.act(sq32, ps[bq][:, :], AF.Square, [('ps', bq)], ['sq32'])
                b.mm(ps[6][:, :], ones, sq32, True, True, ['cst', 'sq32'], [('ps', 6)])
                b.mm(ps[7][:, :], self.RT, y32, True, True, ['cst', 'y32'], [('ps', 7)])
                b.act(rs, ps[6][:, :], AF.Sqrt, [('ps', 6), 'cst'], ['rs'], bias=self.eps6, scale=1.0 / 128)
                b.recip(rs, rs, ['rs'], ['rs'])
                b.tt('pool', t1, y32, rC, ALU.mult, ['y32', 'rC'], ['t1'])
                b.tt('dve', sq32, ps[7][:, :], rS, ALU.mult, [('ps', 7), 'rS'], ['sq32'])
                b.tt('pool', t1, t1, sq32, ALU.add, ['t1', 'sq32'], ['t1'])
                if hc < 8:
                    b.tt('dve', qT[:, hc, sl], t1, rs, ALU.mult, ['t1', 'rs'], [('qT', hc, t)])
                else:
                    kv = hc - 8
                    b.tt('dve', kT[:, kv, 256 + t * TT:256 + (t + 1) * TT], t1, rs, ALU.mult, ['t1', 'rs'],
                         [('kT', kv, 2 + t * 4 + q) for q in range(4)])
        scale = 128.0 ** -0.5
        it = 0
        pit = 0
        for h in range(8):
            kv = h // 4
            for qp in range(4):
                bo = it % 2
                bd = 2 + it % 2
                it += 1
                q0 = qp * TT
                for sbi in range(18):
                    bs = 4 + pit % 3
                    p = pT[pit % 3]
                    pk = ('pT', pit % 3)
                    pit += 1
                    for hh in range(2):
                        b.mm(ps[bs][:, hh * 256:(hh + 1) * 256], kT[:, kv, sbi * 128:(sbi + 1) * 128],
                             qT[:, h, q0 + hh * 256:q0 + (hh + 1) * 256], True, True,
                             [('kT', kv, sbi), ('qT', h, qp)], [('ps', bs)])
                    for hh in range(2):
                        c = sbi * 8 + qp * 2 + hh
                        b.act(p[:, hh * 256:(hh + 1) * 256], ps[bs][:, hh * 256:(hh + 1) * 256], AF.Exp,
                              [('ps', bs), 'small'], [pk], bias=self.attb_sb[:, c:c + 1], scale=scale)
                    b.mm(ps[bo][:, :], vS[:, sbi, kv * 128:(kv + 1) * 128], p, sbi == 0, sbi == 17,
                         [('vS', sbi), pk], [('ps', bo)])
                    b.mm(ps[bd][:, :], onesb, p, sbi == 0, sbi == 17, ['cstb', pk], [('ps', bd)])
                b.recip(rden, ps[bd][:, :], [('ps', bd)], ['rden'])
                b.tt('dve', hT[:, h, q0:q0 + TT], ps[bo][:, :], rden, ALU.mult, [('ps', bo), 'rden'], [('hT', h, qp)])
        for kc in range(KC):
            b.dma('pool', W[:, kc, 0:1024], self.w_o[i, kc * 128:(kc + 1) * 128, :], (), [('W', kc)])
        for m in range(KC):
            for t in range(NT):
                sl = slice(t * TT, (t + 1) * TT)
                bk = 4 + (m * NT + t) % 4
                for hh in range(8):
                    b.mm(ps[bk][:, :], W[:, hh, m * 128:(m + 1) * 128], hT[:, hh, sl], hh == 0, hh == 7,
                         [('W', hh), ('hT', hh, t)], [('ps', bk)])
                xk = ('xT', m, t)
                b.stt(self.xT[:, m, sl], ps[bk][:, :], self.modcol(l, 2, m), self.xT[:, m, sl], ALU.mult, ALU.add,
                      [('ps', bk), xk, 'mods'], [xk])
        b.emit()
        self.post_ln_block(l)


def _bf(a):
    return np.ascontiguousarray(a.astype(ml_dtypes.bfloat16))


def _fm(v):
    v = np.asarray(v, np.float32)
    n = v.shape[-1] // 128
    lead = v.shape[:-1]
    r = v.reshape(lead + (n, 128))
    r = np.moveaxis(r, -1, 0)
    return np.ascontiguousarray(r.reshape(128, -1))


def _const_tables():
    cst = np.zeros((128, 512), np.float32)
    cst[:, 0:128] = np.eye(128, dtype=np.float32)
    RT = np.zeros((128, 128), np.float32)
    for i in range(64):
        RT[2 * i + 1, 2 * i] = -1.0
        RT[2 * i, 2 * i + 1] = 1.0
    cst[:, 128:256] = RT
    cst[:, 256:384] = 1.0
    cst[:, 384] = 1e-5
    cst[:, 385] = 1e-6
    cb = np.zeros((8, 128, 128), np.float32)
    cb[0] = 0.0
    cb[0][0, :] = 1.0
    j = np.arange(128)[:, None]
    i = np.arange(128)[None, :]
    same = (j // 64) == (i // 64)
    cb[1] = same & (j <= i)
    cb[2] = same & (j >= i)
    cb[3] = same & (j > i)
    cb[4] = same & (j < i)
    cb[5] = same & (j <= i)
    cb[6] = same & (j >= i)
    cb[7] = 1.0
    cstb = _bf(np.concatenate(list(cb), axis=1))
    c = np.arange(128)
    ang = 2.0 * np.pi * np.outer(c, c) / 128.0
    cs128 = _bf(np.concatenate([np.cos(ang), np.sin(ang)], axis=1) / np.sqrt(128.0))
    return cst, cstb, cs128


def _dft_tables(tseq):
    t = np.arange(T)
    blk = t // tseq
    loc = (t % tseq).astype(np.float64)
    ang = 2.0 * np.pi * np.outer(loc, loc) / tseq
    same = (blk[:, None] == blk[None, :])
    Cm = np.where(same, np.cos(ang), 0.0) / np.sqrt(tseq)
    Sm = np.where(same, -np.sin(ang), 0.0) / np.sqrt(tseq)
    return _bf(Cm.astype(np.float32)), _bf(Sm.astype(np.float32))


def _rope_tables(latent):
    if not latent:
        return np.ones((128, T), np.float32), np.zeros((128, T), np.float32)
    t = np.arange(T)
    row = (t // 64).astype(np.float32)
    col = (t % 64).astype(np.float32)
    freqs = (10000.0 ** (-np.arange(32, dtype=np.float32) / 32)).astype(np.float32)
    ang = np.concatenate([row[:, None] * freqs, col[:, None] * freqs], axis=-1)
    cos = np.cos(ang).astype(np.float32)
    sin = np.sin(ang).astype(np.float32)
    C = np.repeat(cos.T, 2, axis=0)
    S = np.repeat(sin.T, 2, axis=0)
    return np.ascontiguousarray(C), np.ascontiguousarray(S)


def _attb(latent):
    a = np.zeros((128, 144), np.float32)
    if not latent:
        for sb in range(18):
            for qt in range(8):
                ok = sb >= 2 and ((sb - 2) // 2 == qt)
                a[:, sb * 8 + qt] = 0.0 if ok else NEG_BIG
    return a


def prep_inputs(inp):
    g = lambda k: np.asarray(inp[k], np.float32)
    cst, cstb, cs128 = _const_tables()
    w_in = g('w_in_even')
    w_in_pad = np.zeros((2, D, 2176), np.float32)
    w_in_pad[:, :, 0:2048] = w_in[:, :, 0:2048]
    w_in_pad[:, :, 2048:2064] = w_in[:, :, 2048:2064]
    w_in_pad[:, :, 2080:2096] = w_in[:, :, 2064:2080]
    ln_g, ln_b = g('ln_g'), g('ln_b')
    lnT = _fm(np.stack([ln_g, ln_b], axis=2))
    shared = dict(
        cst=cst, cstb=cstb, cs128=cs128,
        w_mod=g('w_mod'), b_modT=_fm(g('b_mod')), lnT=lnT,
        w_in=w_in_pad, w_a2=g('w_a2'), b_a=g('b_a'), gng=np.ascontiguousarray(g('gla_norm_g').T),
        w_oe=g('w_out_even'), w_qkv=g('w_qkv'), qng=np.ascontiguousarray(g('q_norm_g').T),
        kng=np.ascontiguousarray(g('k_norm_g').T),
        kng_bc=np.ascontiguousarray(np.broadcast_to(g('k_norm_g').reshape(1, 256), (128, 256))),
        w_o=g('w_o'), w_r=g('w_router'), b_r=g('b_router'), w_gu=g('w_gate_up'),
        b_guT=_fm(g('b_gate_up')), w_d=g('w_down'), b_d=g('b_down'),
    )
    lat = dict(zip(('ropeC', 'ropeS'), _rope_tables(True)))
    lat['attb'] = _attb(True)
    lat['keep'] = np.ones((128, 8), np.float32)
    lat['dftC'], lat['dftS'] = _dft_tables(2048)
    pro = dict(zip(('ropeC', 'ropeS'), _rope_tables(False)))
    pro['attb'] = _attb(False)
    pro['keep'] = np.zeros((128, 8), np.float32)
    pro['dftC'], pro['dftS'] = _dft_tables(256)
    xs, xp = g('x_sample'), g('x_prompt')
    maps = []
    for core in range(8):
        m = dict(shared)
        if core < 4:
            m.update(lat)
            m['x'] = np.ascontiguousarray(xs[core])
            m['cT'] = _fm(g('c')[core])
            m['st0'] = np.ascontiguousarray(g('state_gla')[core])
            m['ck'] = np.ascontiguousarray(g('cache_k')[core].reshape(2, 256, 256))
            m['cv'] = np.ascontiguousarray(g('cache_v')[core].reshape(2, 256, 256))
        else:
            grp = (core - 4) % 2
            m.update(pro)
            m['x'] = np.ascontiguousarray(xp[grp * 8:(grp + 1) * 8].reshape(T, D))
            m['cT'] = _fm(g('c_ctx'))
            m['st0'] = np.zeros((2, 2, 4, 64, 128), np.float32)
            m['ck'] = np.zeros((2, 256, 256), np.float32)
            m['cv'] = np.zeros((2, 256, 256), np.float32)
        maps.append(m)
    return maps


_NC_CACHE = {}


def run(inputs, depth=DEPTH, mixers=True, cores=8):
    key = (depth, mixers)
    if key not in _NC_CACHE:
        _NC_CACHE[key] = Builder(depth, mixers).build()
    nc = _NC_CACHE[key]
    maps = prep_inputs(inputs)[:cores]
    res = run_bass_kernel_spmd(nc, maps, core_ids=list(range(cores)))
    return res.results


def kernel(**inputs):
    r = run(inputs)
    y_sample = np.stack([r[c]['y'] for c in range(4)], axis=0)
    y_prompt = np.concatenate([r[4 + gI]['y'].reshape(8, 256, D) for gI in range(2)], axis=0)
    ns = np.concatenate([np.transpose(r[4 + gI]['ns'], (2, 0, 1, 3, 4, 5)) for gI in range(2)], axis=0)
    nk = np.concatenate([np.transpose(r[4 + gI]['nk'].reshape(2, 8, 256, 2, 128), (1, 0, 2, 3, 4)) for gI in range(2)], axis=0)
    nv = np.concatenate([np.transpose(r[4 + gI]['nv'].reshape(2, 8, 256, 2, 128), (1, 0, 2, 3, 4)) for gI in range(2)], axis=0)
    return (np.ascontiguousarray(y_prompt, np.float32), np.ascontiguousarray(y_sample, np.float32),
            np.ascontiguousarray(ns, np.float32), np.ascontiguousarray(nk, np.float32),
            np.ascontiguousarray(nv, np.float32))
```

```python
import numpy as np
import ml_dtypes
import concourse.bass as bass
import concourse.mybir as mybir
from concourse.bass_utils import run_bass_kernel_spmd

F32 = mybir.dt.float32
BF16 = mybir.dt.bfloat16
AF = mybir.ActivationFunctionType
ALU = mybir.AluOpType
AX = mybir.AxisListType

D = 1024
KC = 8
T = 2048
TT = 512
NT = 4
NB = 16
DEPTH = 4
NE = 32
ALPHA = (2.0 * DEPTH) ** 0.25
NEG_BIG = -30000.0
ARENA_COLS = 52224
SAME_SYNC = True
NDS = 24

ENGS = ('pe', 'act', 'dve', 'pool', 'sp')
BLOCK_NAME = {'pe': 'tensor', 'act': 'scalar', 'dve': 'vector', 'pool': 'gpsimd', 'sp': 'sync'}


class Op:
    __slots__ = ('eng', 'fn', 'deps', 'is_dma', 'sem', 'val', 'prev')

    def __init__(self, eng, fn, deps, is_dma):
        self.eng = eng
        self.fn = fn
        self.deps = deps
        self.is_dma = is_dma
        self.sem = None
        self.val = None
        self.prev = 0


class Sync:
    def __init__(self, nc, stack):
        self.nc = nc
        self.eng_sem = {e: stack.enter_context(nc.semaphore('s_' + e)) for e in ('pe', 'act', 'dve', 'pool')}
        self.eng_cnt = {e: 0 for e in self.eng_sem}
        self.dma_sems = {q: [stack.enter_context(nc.semaphore('d_%s%d' % (q, i))) for i in range(NDS)]
                         for q in ('sp', 'pool')}
        self.dma_cnt = {q: [0] * NDS for q in ('sp', 'pool')}
        self.dma_rr = {q: 0 for q in ('sp', 'pool')}
        self.final_waits = []


class Blk:
    def __init__(self, nc, sync, ps):
        self.nc = nc
        self.sync = sync
        self.ops = []
        self.lw = {}
        self.rd = {}
        self.ps = ps
        self.bank_rr = 0

    def bank(self):
        b = self.bank_rr % 8
        self.bank_rr += 1
        return b

    def add(self, eng, fn, rd=(), wr=(), is_dma=False):
        idx = len(self.ops)
        deps = set()
        for k in rd:
            w = self.lw.get(k)
            if w is not None:
                deps.add(w)
        for k in wr:
            w = self.lw.get(k)
            if w is not None:
                deps.add(w)
            for r in self.rd.get(k, ()):
                deps.add(r)
        for k in rd:
            self.rd.setdefault(k, []).append(idx)
        for k in wr:
            self.lw[k] = idx
            self.rd[k] = []
        last = {}
        keep = []
        for dd in deps:
            od = self.ops[dd]
            if od.is_dma:
                keep.append(dd)
            elif last.get(od.eng, -1) < dd:
                last[od.eng] = dd
        keep.extend(last.values())
        self.ops.append(Op(eng, fn, sorted(keep), is_dma))
        return idx

    def mm(self, out, lhsT, rhs, start, stop, rd, wr):
        self.add('pe', lambda e: e.matmul(out, lhsT=lhsT, rhs=rhs, start=start, stop=stop), rd, wr)

    def tr(self, out, in_, ident, rd, wr):
        self.add('pe', lambda e: e.transpose(out, in_, ident), rd, wr)

    def act(self, out, in_, func, rd, wr, bias=None, scale=None, accum_out=None):
        kw = {}
        if bias is not None:
            kw['bias'] = bias
        if scale is not None:
            kw['scale'] = scale
        if accum_out is not None:
            kw['accum_out'] = accum_out
        self.add('act', lambda e: e.activation(out=out, in_=in_, func=func, **kw), rd, wr)

    def ts(self, eng, out, in0, s1, s2, op0, op1, rd, wr):
        if op1 is None:
            self.add(eng, lambda e: e.tensor_scalar(out=out, in0=in0, scalar1=s1, scalar2=None, op0=op0), rd, wr)
        else:
            self.add(eng, lambda e: e.tensor_scalar(out=out, in0=in0, scalar1=s1, scalar2=s2, op0=op0, op1=op1), rd, wr)

    def tt(self, eng, out, in0, in1, op, rd, wr):
        self.add(eng, lambda e: e.tensor_tensor(out=out, in0=in0, in1=in1, op=op), rd, wr)

    def stt(self, out, in0, scalar, in1, op0, op1, rd, wr):
        self.add('dve', lambda e: e.scalar_tensor_tensor(out=out, in0=in0, scalar=scalar, in1=in1, op0=op0, op1=op1), rd, wr)

    def cp(self, eng, out, in_, rd, wr):
        if eng == 'act':
            self.add('act', lambda e: e.activation(out=out, in_=in_, func=AF.Copy), rd, wr)
        else:
            self.add(eng, lambda e: e.tensor_copy(out=out, in_=in_), rd, wr)

    def recip(self, out, in_, rd, wr):
        self.add('dve', lambda e: e.reciprocal(out=out, in_=in_), rd, wr)

    def dma(self, q, out, in_, rd, wr):
        return self.add(q, lambda e: e.dma_start(out=out, in_=in_), rd, wr, is_dma=True)

    def emit(self, final=False):
        ops = self.ops
        sy = self.sync
        n = len(ops)
        signal = [False] * n
        for o in ops:
            for d in o.deps:
                od = ops[d]
                if od.is_dma:
                    continue
                if od.eng != o.eng or (SAME_SYNC and o.eng != 'pe'):
                    signal[d] = True
        for i, o in enumerate(ops):
            if o.is_dma:
                q = o.eng
                k = sy.dma_rr[q] % NDS
                sy.dma_rr[q] += 1
                o.sem = sy.dma_sems[q][k]
                o.prev = sy.dma_cnt[q][k]
                sy.dma_cnt[q][k] += 16
                o.val = sy.dma_cnt[q][k]
            elif signal[i]:
                sy.eng_cnt[o.eng] += 1
                o.val = sy.eng_cnt[o.eng]
                o.sem = sy.eng_sem[o.eng]
        out_dmas = [o for o in ops if o.is_dma and getattr(o, 'is_out', False)]
        with self.nc.Block() as block:
            for eng in ENGS:
                my = [o for o in ops if o.eng == eng]
                if not my and not (final and eng == 'sp'):
                    continue

                def body(e, eng=eng, my=my):
                    waited = {}

                    def w(sem, val):
                        k = id(sem)
                        if waited.get(k, 0) < val:
                            e.wait_ge(sem, val)
                            waited[k] = val
                    for o in my:
                        if o.is_dma and o.prev > 0:
                            w(o.sem, o.prev)
                        for d in o.deps:
                            od = ops[d]
                            if od.is_dma:
                                w(od.sem, od.val)
                            elif od.eng != eng or (SAME_SYNC and eng != 'pe'):
                                w(od.sem, od.val)
                        ins = o.fn(e)
                        if o.is_dma:
                            ins.then_inc(o.sem, 16)
                        elif o.val is not None:
                            ins.then_inc(o.sem, 1)
                    if eng == 'sp' and final:
                        for q in ('sp', 'pool'):
                            for k in range(NDS):
                                if sy.dma_cnt[q][k] > 0:
                                    w(sy.dma_sems[q][k], sy.dma_cnt[q][k])
                getattr(block, BLOCK_NAME[eng])(body)


class Arena:
    def __init__(self, ap, base):
        self.ap = ap
        self.off = base
        self.base = base

    def f32(self, ncols, shape=None):
        a = self.ap[:, self.off:self.off + ncols]
        self.off += ncols
        assert self.off <= ARENA_COLS, self.off
        return a

    def f32v(self, dims):
        n = int(np.prod(dims))
        a = self.f32(n)
        if len(dims) == 2:
            return a.rearrange("p (a b) -> p a b", a=dims[0])
        return a

    def bf(self, dims):
        n = int(np.prod(dims))
        assert n % 2 == 0
        a = self.f32(n // 2).bitcast(BF16)
        if len(dims) == 2:
            return a.rearrange("p (a b) -> p a b", a=dims[0])
        if len(dims) == 3:
            return a.rearrange("p (a b c) -> p a b c", a=dims[0], b=dims[1])
        return a


C_IDENT, C_RT, C_ONES, C_EPS5, C_EPS6 = 0, 128, 256, 384, 385
C_CSTB = 512
C_MOD = 1024
C_LN = 1216
C_QNG, C_KNG, C_GNG = 1344, 1346, 1348
C_ATTB = 1352
C_KEEP = 1496
C_SCB = 1504
C_CT = 1508
C_BMODT = 1520
PERSIST = 16384 + 2048


class Builder:
    def __init__(self, depth=DEPTH, mixers=True, ne_run=NE, moe=True, layers=None, dbg_stop=99, dbg_sub=99):
        self.dbg_stop = dbg_stop
        self.dbg_sub = dbg_sub
        self.layers = list(range(depth)) if layers is None else layers
        self.depth = depth
        self.mixers = mixers
        self.ne_run = ne_run
        self.moe = moe

    def dram(self, name, shape, dt=F32, out=False):
        return self.nc.dram_tensor(name, list(shape), dt, kind="ExternalOutput" if out else "ExternalInput").ap()

    def build(self):
        from contextlib import ExitStack
        nc = bass.Bass("TRN2", target_bir_lowering=False)
        self.nc = nc
        d = self.dram
        self.x = d("x", [T, D])
        self.cT = d("cT", [128, 8])
        self.st0 = d("st0", [2, 2, 4, 64, 128])
        self.ck = d("ck", [2, 256, 256])
        self.cv = d("cv", [2, 256, 256])
        self.ropeC = d("ropeC", [128, T])
        self.ropeS = d("ropeS", [128, T])
        self.attb = d("attb", [128, 144])
        self.keep = d("keep", [128, 8])
        self.dftC = d("dftC", [T, T], BF16)
        self.dftS = d("dftS", [T, T], BF16)
        self.cs128 = d("cs128", [128, 256], BF16)
        self.cst = d("cst", [128, 512])
        self.cstb = d("cstb", [128, 1024], BF16)
        self.w_mod = d("w_mod", [4, D, 6 * D])
        self.b_modT = d("b_modT", [128, 192])
        self.lnT = d("lnT", [128, 128])
        self.w_in = d("w_in", [2, D, 2176])
        self.w_a2 = d("w_a2", [2, 2, 16, 256])
        self.b_a = d("b_a", [2, 2, 256])
        self.gng = d("gng", [128, 2])
        self.w_oe = d("w_oe", [2, D, D])
        self.w_qkv = d("w_qkv", [2, D, 1536])
        self.qng = d("qng", [128, 2])
        self.kng = d("kng", [128, 2])
        self.kng_bc = d("kng_bc", [128, 256])
        self.w_o = d("w_o", [2, D, D])
        self.w_r = d("w_r", [4, D, NE])
        self.b_r = d("b_r", [4, NE])
        self.w_gu = d("w_gu", [4, NE, D, 2 * D] if self.moe else [1, 1, 128, 128])
        self.b_guT = d("b_guT", [128, 4 * NE * 16])
        self.w_d = d("w_d", [4, NE, D, D] if self.moe else [1, 1, 128, 128])
        self.b_d = d("b_d", [4, NE, D])
        self.y = d("y", [T, D], out=True)
        self.ns = d("ns", [2, 2, 8, 4, 64, 128], out=True)
        self.nk = d("nk", [2, T, 256], out=True)
        self.nv = d("nv", [2, T, 256], out=True)

        with ExitStack() as stack:
            arena = stack.enter_context(nc.sbuf_tensor("arena", [128, ARENA_COLS], F32))
            self.arena = arena[:, :]
            self.ps = [stack.enter_context(nc.psum_tensor("ps%d" % i, [128, 512], F32)) for i in range(8)]
            self.sync = Sync(nc, stack)
            A = self.arena
            self.xT = A[:, 0:16384].rearrange("p (a b) -> p a b", a=8)
            P0 = 16384
            self.cc = lambda off, n: A[:, P0 + off:P0 + off + n]
            self.ident = self.cc(C_IDENT, 128)
            self.RT = self.cc(C_RT, 128)
            self.ones = self.cc(C_ONES, 128)
            self.eps5 = self.cc(C_EPS5, 1)
            self.eps6 = self.cc(C_EPS6, 1)
            cb = self.cc(C_CSTB, 512).bitcast(BF16)
            self.cb = lambda i: cb[:, i * 128:(i + 1) * 128]
            self.mod = self.cc(C_MOD, 192)
            self.lnp = self.cc(C_LN, 128)
            self.attb_sb = self.cc(C_ATTB, 144)
            self.keep_sb = self.cc(C_KEEP, 8)

            self.prologue()
            for l in self.layers:
                if self.mixers:
                    if l % 2 == 0:
                        self.even_layer(l)
                    else:
                        self.odd_layer(l)
                else:
                    self.nomix_layer(l)
                for half in range(2):
                    if self.moe:
                        self.moe_router(l, half)
                        self.moe_experts(l, half)
            self.epilogue()
            self.counts = dict(self.sync.eng_cnt)
        return nc

    def newblk(self):
        return Blk(self.nc, self.sync, self.ps)

    def prologue(self):
        b = self.newblk()
        ps = self.ps
        A = self.arena
        P0 = 16384
        b.dma('sp', self.cc(0, 512), self.cst[:, :], (), ['cst'])
        b.dma('sp', self.cc(C_CSTB, 512).bitcast(BF16), self.cstb[:, :], (), ['cstb'])
        b.dma('sp', self.lnp, self.lnT[:, :], (), ['lnp'])
        b.dma('sp', self.cc(C_QNG, 2), self.qng[:, :], (), ['small'])
        b.dma('sp', self.cc(C_KNG, 2), self.kng[:, :], (), ['small'])
        b.dma('sp', self.cc(C_GNG, 2), self.gng[:, :], (), ['small'])
        b.dma('sp', self.attb_sb, self.attb[:, :], (), ['small'])
        b.dma('sp', self.keep_sb, self.keep[:, :], (), ['small'])
        b.dma('sp', self.cc(C_CT, 8), self.cT[:, :], (), ['cT'])
        b.dma('sp', self.cc(C_BMODT, 192), self.b_modT[:, :], (), ['bmodT'])
        ar = Arena(A, PERSIST)
        xs = [ar.f32(1024) for _ in range(2)]
        for tb in range(NB):
            s = xs[tb % 2]
            b.dma('sp', s, self.x[tb * 128:(tb + 1) * 128, :], (), [('xs', tb % 2)])
            for half in range(2):
                bk = b.bank()
                for c in range(4):
                    kc = half * 4 + c
                    b.tr(ps[bk][:, c * 128:(c + 1) * 128], s[:, kc * 128:(kc + 1) * 128], self.ident,
                         [('xs', tb % 2), 'cst'], [('ps', bk)])
                b.cp('act' if half == 0 else 'dve',
                     self.xT[:, half * 4:half * 4 + 4, tb * 128:(tb + 1) * 128],
                     ps[bk][:, :].rearrange("p (a b) -> p a b", a=4),
                     [('ps', bk)], [('xT', kc, tb // 4) for kc in range(half * 4, half * 4 + 4)])
        scb = self.cc(C_SCB, 4).bitcast(BF16)
        b.act(scb, self.cc(C_CT, 8), AF.Silu, ['cT'], ['scb'])
        wm = [ar.bf([8, 3072]) for _ in range(2)]
        bkM = b.bank()
        it = 0
        for l in range(self.depth):
            for half in range(2):
                w = wm[it % 2]
                for kc in range(KC):
                    b.dma('pool', w[:, kc, :], self.w_mod[l, kc * 128:(kc + 1) * 128, half * 3072:(half + 1) * 3072],
                          (), [('wm', it % 2, kc)])
                for oc in range(24):
                    col = half * 24 + oc
                    for kc in range(KC):
                        b.mm(ps[bkM][:, col:col + 1], w[:, kc, oc * 128:(oc + 1) * 128], scb[:, kc:kc + 1],
                             kc == 0, kc == KC - 1, [('wm', it % 2, kc), 'scb'], [('ps', bkM)])
                it += 1
            b.tt('dve', self.mod[:, l * 48:(l + 1) * 48], ps[bkM][:, 0:48], self.cc(C_BMODT + l * 48, 48), ALU.add,
                 [('ps', bkM), 'bmodT'], [('mod', l)])
            for s in (1, 4):
                sl = self.mod[:, l * 48 + s * 8:l * 48 + s * 8 + 8]
                b.ts('dve', sl, sl, 1.0, None, ALU.add, None, [('mod', l)], [('mod', l)])
        b.emit()

    def ln(self, b, ar, tiles, scale_of, shift_of, eps, out_of, out_keys_of, extra=None, prescale=None,
           src_keys=None):
        ps = self.ps
        sqb = [ar.f32(TT) for _ in range(2)]
        tmp = [ar.f32(TT) for _ in range(4)]
        mean = ar.f32(TT)
        msq = ar.f32(TT)
        rstd = ar.f32(TT)
        nmr = ar.f32(TT)
        for t in tiles:
            sl = slice(t * TT, (t + 1) * TT)
            bm = b.bank()
            bq = b.bank()
            for kc in range(KC):
                xk = ('xT', kc, t)
                sq = sqb[kc % 2]
                b.act(sq, self.xT[:, kc, sl], AF.Square, [xk], [('sqb', kc % 2)])
                b.mm(ps[bm][:, :], self.ones, self.xT[:, kc, sl], kc == 0, kc == KC - 1, [xk, 'cst'], [('ps', bm)])
                b.mm(ps[bq][:, :], self.ones, sq, kc == 0, kc == KC - 1, [('sqb', kc % 2), 'cst'], [('ps', bq)])
            b.act(mean, ps[bm][:, :], AF.Copy, [('ps', bm)], ['ln_mean'], scale=1.0 / D)
            b.tt('dve', msq, mean, mean, ALU.mult, ['ln_mean'], ['ln_msq'])
            b.stt(msq, ps[bq][:, :], 1.0 / D, msq, ALU.mult, ALU.subtract, [('ps', bq), 'ln_msq'], ['ln_msq'])
            b.act(msq, msq, AF.Sqrt, ['ln_msq', 'cst'], ['ln_msq'], bias=eps)
            b.recip(rstd, msq, ['ln_msq'], ['ln_rstd'])
            b.stt(nmr, mean, -1.0, rstd, ALU.mult, ALU.mult, ['ln_mean', 'ln_rstd'], ['ln_nmr'])
            for kc in range(KC):
                xk = ('xT', kc, t)
                tp = tmp[kc % 4]
                tk = ('lntmp', kc % 4)
                b.tt('dve', tp, self.xT[:, kc, sl], rstd, ALU.mult, [xk, 'ln_rstd'], [tk])
                b.tt('dve', tp, tp, nmr, ALU.add, [tk, 'ln_nmr'], [tk])
                b.act(out_of(kc, t), tp, AF.Identity, [tk, 'mods'], out_keys_of(kc, t),
                      bias=shift_of(kc), scale=scale_of(kc))
                if extra is not None:
                    eo, ek = extra(kc, t)
                    b.act(eo, tp, AF.Identity, [tk, 'mods'], ek, bias=shift_of(kc), scale=scale_of(kc))
                if prescale is not None:
                    b.act(self.xT[:, kc, sl], self.xT[:, kc, sl], AF.Copy, [xk], [xk], scale=float(prescale))

    def modcol(self, l, s, kc):
        i = l * 48 + s * 8 + kc
        return self.mod[:, i:i + 1]

    def lncol(self, l, which, gb, kc):
        i = ((l * 2 + which) * 2 + gb) * 8 + kc
        return self.lnp[:, i:i + 1]

    def nomix_layer(self, l):
        b = self.newblk()
        ar = Arena(self.arena, PERSIST)
        for kc in range(KC):
            for t in range(NT):
                sl = slice(t * TT, (t + 1) * TT)
                b.ts('pool', self.xT[:, kc, sl], self.xT[:, kc, sl], float(ALPHA), None, ALU.mult, None,
                     [('xT', kc, t)], [('xT', kc, t)])
        self.ln(b, ar, range(NT), lambda kc: self.lncol(l, 0, 0, kc), lambda kc: self.lncol(l, 0, 1, kc), self.eps5,
                lambda kc, t: self.xT[:, kc, t * TT:(t + 1) * TT], lambda kc, t: [('xT', kc, t)])
        b.emit()

    def moe_router(self, l, half):
        b = self.newblk()
        ps = self.ps
        ar = Arena(self.arena, PERSIST)
        self.hT = ar.bf([8, 1024])
        self.combT = ar.bf([1024])
        h32s = [ar.f32v([8, TT]) for _ in range(2)]
        wr = ar.f32v([8, NE])
        br = ar.f32(NE)
        lg2 = [ar.f32(NE) for _ in range(2)]
        m82 = [ar.f32(8) for _ in range(2)]
        nmax2 = [ar.f32(1) for _ in range(2)]
        ex2 = [ar.f32(NE) for _ in range(2)]
        mask2 = [ar.f32(NE) for _ in range(2)]
        ssum2 = [ar.f32(1) for _ in range(2)]
        comb2 = [ar.f32(NE) for _ in range(2)]
        b.dma('sp', wr, self.w_r[l].rearrange("(kc p) n -> p kc n", p=128), (), ['wr'])
        b.dma('sp', br[0:1, :], self.b_r[l:l + 1, :], (), ['br'])
        tiles = [half * 2, half * 2 + 1]

        def out_of(kc, t):
            tl = t - half * 2
            return self.hT[:, kc, tl * TT:(tl + 1) * TT]

        lnar = Arena(self.arena, ar.off)
        first = True
        for t in tiles:
            tl = t - half * 2
            sub = Arena(self.arena, lnar.base)
            h32 = h32s[tl]
            self.ln(b, sub, [t], lambda kc: self.modcol(l, 4, kc), lambda kc: self.modcol(l, 3, kc), self.eps5,
                    out_of, lambda kc, t: [('hT', kc, t - half * 2)],
                    extra=lambda kc, t, h32=h32, tl=tl: (h32[:, kc, :], [('h32', kc, tl)]), prescale=ALPHA)
            for q in range(4):
                tbl = tl * 4 + q
                r2 = tbl % 2
                lg, m8, nmax, ex, mask, ssum, comb = lg2[r2], m82[r2], nmax2[r2], ex2[r2], mask2[r2], ssum2[r2], comb2[r2]
                K_ = lambda n: (n, r2)
                bk = b.bank()
                b.mm(ps[bk][:, 0:NE], self.ones[0:1, 0:128], br[0:1, :], True, False, ['cst', 'br'], [('ps', bk)])
                for kc in range(KC):
                    b.mm(ps[bk][:, 0:NE], h32[:, kc, q * 128:(q + 1) * 128], wr[:, kc, :], False, kc == KC - 1,
                         [('h32', kc, tl), 'wr'], [('ps', bk)])
                b.cp('act', lg, ps[bk][:, 0:NE], [('ps', bk)], [K_('lg')])
                b.add('dve', lambda e, m8=m8, lg=lg: e.max(out=m8, in_=lg), [K_('lg')], [K_('m8')])
                b.ts('dve', nmax, m8[:, 0:1], -1.0, None, ALU.mult, None, [K_('m8')], [K_('nmax')])
                b.act(ex, lg, AF.Exp, [K_('lg'), K_('nmax')], [K_('ex')], bias=nmax)
                b.ts('dve', mask, lg, m8[:, 3:4], None, ALU.is_ge, None, [K_('lg'), K_('m8')], [K_('mask')])
                b.tt('dve', ex, ex, mask, ALU.mult, [K_('ex'), K_('mask')], [K_('ex')])
                b.add('dve', lambda e, ssum=ssum, ex=ex: e.reduce_sum(out=ssum, in_=ex, axis=AX.X), [K_('ex')], [K_('ssum')])
                b.recip(ssum, ssum, [K_('ssum')], [K_('ssum')])
                b.ts('dve', comb, ex, ssum, None, ALU.mult, None, [K_('ex'), K_('ssum')], [K_('comb')])
                bk2 = b.bank()
                b.tr(ps[bk2][0:NE, 0:128], comb, self.ident, [K_('comb'), 'cst'], [('ps', bk2)])
                b.cp('act', self.combT[0:NE, tbl * 128:(tbl + 1) * 128], ps[bk2][0:NE, 0:128], [('ps', bk2)],
                     [('combT', tbl)])
        self.moe_off = ar.off - 0
        b.emit()

    def moe_experts(self, l, half):
        b = self.newblk()
        ps = self.ps
        ar = Arena(self.arena, PERSIST)
        hT = ar.bf([8, 1024])
        combT = ar.bf([1024])
        actT = ar.bf([8, 1024])
        WGa = [ar.bf([4, 2048]) for _ in range(2)]
        WGb = ar.bf([4, 2048])
        WD = ar.bf([8, 1024])
        comb_b = ar.bf([1024])
        cmask = ar.bf([1024])
        gb = [ar.f32(TT) for _ in range(3)]
        sb = [ar.f32(TT) for _ in range(3)]
        ub = [ar.f32(TT) for _ in range(3)]
        bgu = ar.f32(512)
        bd = ar.bf([1024])
        ident = self.ident
        onesb = self.cb(7)
        b.dma('sp', bgu, self.b_guT[:, l * 512:(l + 1) * 512], (), ['bgu'])
        b.dma('pool', bd[0:NE, :], self.b_d[l], (), ['bd'])
        bgu3 = bgu.rearrange("p (e c) -> p e c", e=NE)
        b.ts('dve', bgu3[:, :, 8:16], bgu3[:, :, 8:16], 1.0, None, ALU.add, None, ['bgu'], ['bgu'])
        g2 = lambda m: self.modcol(l, 5, m)

        def wgu(e, kc):
            return WGa[e % 2][:, kc, :] if kc < 4 else WGb[:, kc - 4, :]

        def wkey(e, kc):
            return ('wgu_a', e % 2) if kc < 4 else 'wgu_b'

        def load_gu_a(e):
            b.dma('pool', WGa[e % 2], self.w_gu[l, e, 0:512, :].rearrange("(kc p) n -> p kc n", p=128), (),
                  [wkey(e, 0)])

        def load_gu_b(e):
            b.dma('pool', WGb, self.w_gu[l, e, 512:1024, :].rearrange("(kc p) n -> p kc n", p=128), (),
                  [wkey(e, 4)])

        def load_d(e):
            b.dma('pool', WD, self.w_d[l, e].rearrange("(jc p) n -> p jc n", p=128), (), ['wd'])

        def comb_prep(e):
            b.ts('dve', cmask[0:NE, :], combT[0:NE, :], ident[0:NE, e:e + 1], None, ALU.mult, None,
                 [('combT', i) for i in range(8)] + ['cst'], ['cmask'])
            for t in range(2):
                bk = b.bank()
                b.mm(ps[bk][:, :], onesb[0:NE, :], cmask[0:NE, t * TT:(t + 1) * TT], True, True,
                     ['cmask', 'cstb'], [('ps', bk)])
                b.cp('act', comb_b[:, t * TT:(t + 1) * TT], ps[bk][:, :], [('ps', bk)], [('comb_b', t)])

        load_gu_a(0)
        load_gu_b(0)
        load_d(0)
        comb_prep(0)
        it = 0
        pend = None
        NEr = self.ne_run
        for e in range(NEr):
            if e + 1 < NEr:
                load_gu_a(e + 1)
            for j in range(KC):
                for t in range(2):
                    sl = slice(t * TT, (t + 1) * TT)
                    bg = b.bank()
                    bu = b.bank()
                    for kc in range(KC):
                        b.mm(ps[bg][:, :], wgu(e, kc)[:, j * 128:(j + 1) * 128], hT[:, kc, sl], kc == 0, kc == KC - 1,
                             [wkey(e, kc), ('hT', kc, t)], [('ps', bg)])
                    for kc in range(KC):
                        b.mm(ps[bu][:, :], wgu(e, kc)[:, D + j * 128:D + (j + 1) * 128], hT[:, kc, sl], kc == 0,
                             kc == KC - 1, [wkey(e, kc), ('hT', kc, t)], [('ps', bu)])
                    i = it % 3
                    it += 1
                    G, S, U = gb[i], sb[i], ub[i]
                    cg = e * 16 + j
                    cu = e * 16 + 8 + j
                    b.ts('dve', G, ps[bg][:, :], bgu[:, cg:cg + 1], 7.0, ALU.add, ALU.min, [('ps', bg), 'bgu'], [('G', i)])
                    b.ts('dve', U, ps[bu][:, :], bgu[:, cu:cu + 1], 8.0, ALU.add, ALU.min, [('ps', bu), 'bgu'], [('U', i)])
                    b.act(S, G, AF.Sigmoid, [('G', i)], [('S', i)], scale=1.702)
                    b.tt('pool', S, G, S, ALU.mult, [('G', i), ('S', i)], [('S', i)])
                    if pend is not None:
                        pend()

                    def tail(S=S, U=U, i=i, j=j, sl=sl, t=t):
                        b.stt(U, U, -6.0, S, ALU.max, ALU.mult, [('U', i), ('S', i)], [('U', i)])
                        b.tt('pool', actT[:, j, sl], U, comb_b[:, sl], ALU.mult, [('U', i), ('comb_b', t)], [('actT', j, t)])
                    pend = tail
            pend()
            pend = None
            if e + 1 < NEr:
                load_gu_b(e + 1)
            for t in range(2):
                if t == 1 and e + 1 < NEr:
                    comb_prep(e + 1)
                for m in range(KC):
                    sl = slice(t * TT, (t + 1) * TT)
                    gsl = slice((half * 2 + t) * TT, (half * 2 + t + 1) * TT)
                    bk = b.bank()
                    if e == 0:
                        b.mm(ps[bk][:, :], bd[0:NE, m * 128:(m + 1) * 128], combT[0:NE, sl], True, False,
                             ['bd'] + [('combT', t * 4 + q) for q in range(4)], [('ps', bk)])
                    for j in range(KC):
                        b.mm(ps[bk][:, :], WD[:, j, m * 128:(m + 1) * 128], actT[:, j, sl], (j == 0 and e != 0),
                             j == KC - 1, ['wd', ('actT', j, t)], [('ps', bk)])
                    xk = ('xT', m, half * 2 + t)
                    b.stt(self.xT[:, m, gsl], ps[bk][:, :], g2(m), self.xT[:, m, gsl], ALU.mult, ALU.add,
                          [('ps', bk), xk, 'mods'], [xk])
            if e + 1 < NEr:
                load_d(e + 1)
        b.emit()
        b = self.newblk()
        ar = Arena(self.arena, PERSIST)
        self.ln(b, ar, [half * 2, half * 2 + 1], lambda kc: self.lncol(l, 1, 0, kc), lambda kc: self.lncol(l, 1, 1, kc),
                self.eps5, lambda kc, t: self.xT[:, kc, t * TT:(t + 1) * TT], lambda kc, t: [('xT', kc, t)])
        b.emit()

    def epilogue(self):
        b = self.newblk()
        ps = self.ps
        ar = Arena(self.arena, PERSIST)
        ys = [ar.f32(512) for _ in range(4)]
        it = 0
        for tb in range(NB):
            for half in range(2):
                bk = b.bank()
                for c in range(4):
                    kc = half * 4 + c
                    b.tr(ps[bk][:, c * 128:(c + 1) * 128], self.xT[:, kc, tb * 128:(tb + 1) * 128], self.ident,
                         [('xT', kc, tb // 4), 'cst'], [('ps', bk)])
                s = ys[it % 4]
                b.cp('act' if it % 2 == 0 else 'dve', s, ps[bk][:, :], [('ps', bk)], [('ys', it % 4)])
                b.dma('sp', self.y[tb * 128:(tb + 1) * 128, half * 512:(half + 1) * 512], s, [('ys', it % 4)], [])
                it += 1
        b.emit(final=True)


    def even_layer(self, l):
        i = l // 2
        A = self.arena
        ps = self.ps
        ident, ones, onesb = self.ident, self.ones, self.cb(7)
        OUTS = self.OUTS

        def region(off, dims):
            a = Arena(A, PERSIST + off)
            return a.bf(dims)

        ufT = region(0, [4, 2048])
        qT = region(4096, [2, 2048])
        kT = region(6144, [2, 2048])
        sgT = region(8192, [4, 2048])
        alrT = region(12288, [2048])
        k_tm = region(13312, [16, 256])
        v_tm = region(15360, [16, 512])
        C_EL = 1712
        elast = self.cc(C_EL, 128)

        self.pre_ln_block(l, OUTS)

        b = self.newblk()
        ar = Arena(A, PERSIST + OUTS)
        hT = ar.bf([8, 2048])
        Wp = [ar.bf([8, 512]) for _ in range(2)]

        def load_piece(p):
            W = Wp[p % 2]
            ncol = 512 if p < 4 else 128
            for kc in range(KC):
                b.dma('pool', W[:, kc, 0:ncol], self.w_in[i, kc * 128:(kc + 1) * 128, p * 512:p * 512 + ncol], (),
                      [('Wp', p % 2, kc)])
            return W

        ev = [0]

        def fm(W, pidx, c0, evac):
            for t in range(NT):
                sl = slice(t * TT, (t + 1) * TT)
                bk = b.bank()
                for kc in range(KC):
                    b.mm(ps[bk][:, :], W[:, kc, c0:c0 + 128], hT[:, kc, sl], kc == 0, kc == KC - 1,
                         [('Wp', pidx % 2, kc), ('hT', kc, t)], [('ps', bk)])
                evac(bk, t, sl)

        def tm(W, pidx, c0, n, dst, dkey):
            for tb in range(NB):
                bk = b.bank()
                for kc in range(KC):
                    b.mm(ps[bk][:, 0:n], hT[:, kc, tb * 128:(tb + 1) * 128], W[:, kc, c0:c0 + n], kc == 0,
                         kc == KC - 1, [('Wp', pidx % 2, kc), ('hT', kc, tb // 4)], [('ps', bk)])
                ev[0] += 1
                b.cp('act' if ev[0] % 2 else 'dve', dst[:, tb, :], ps[bk][:, 0:n], [('ps', bk)], [(dkey, tb)])

        def cp_to(dst3, key):
            def f(bk, t, sl):
                ev[0] += 1
                b.cp('act' if ev[0] % 2 else 'dve', dst3(sl), ps[bk][:, :], [('ps', bk)], [(key, t)])
            return f

        W = load_piece(0)
        W1 = load_piece(1)
        for g in range(4):
            fm(W, 0, g * 128, cp_to(lambda sl, g=g: ufT[:, g, sl], ('ufT', g)))
        W = W1
        for c in range(2):
            def evq(bk, t, sl, c=c):
                b.act(qT[:, c, sl], ps[bk][:, :], AF.Copy, [('ps', bk)], [('qT', c, t)], scale=0.125)
            fm(W, 1, c * 128, evq)
            fm(W, 1, 256 + c * 128, cp_to(lambda sl, c=c: kT[:, c, sl], ('kT', c)))
        tm(W, 1, 256, 256, k_tm, 'k_tm')
        W = load_piece(2)
        tm(W, 2, 0, 512, v_tm, 'v_tm')
        W = load_piece(3)
        for h in range(4):
            def evg(bk, t, sl, h=h):
                b.act(sgT[:, h, sl], ps[bk][:, :], AF.Silu, [('ps', bk)], [('sgT', h, t)])
            fm(W, 3, h * 128, evg)
        W = load_piece(4)
        fm(W, 4, 0, cp_to(lambda sl: alrT[:, sl], 'alrT'))
        b.emit()

        if self.dbg_stop <= 2:
            return self.post_ln_block(l)
        b = self.newblk()
        ar = Arena(A, PERSIST + OUTS)
        AB = ar.bf([16, 4, 256])
        Ctab = ar.bf([16, 256])
        Stab = ar.bf([16, 256])
        cs = ar.bf([256])
        b.dma('sp', cs, self.cs128[:, :], (), ['cs'])
        for tb in range(NB):
            for gp in range(2):
                bk = 4 + (tb * 2 + gp) % 4
                for gg in range(2):
                    g = gp * 2 + gg
                    b.mm(ps[bk][:, gg * 256:(gg + 1) * 256], ufT[:, g, tb * 128:(tb + 1) * 128], cs, True, True,
                         [(('ufT', g), tb // 4), 'cs'], [('ps', bk)])
                b.cp('act' if gp == 0 else 'dve', AB[:, tb, gp * 2:gp * 2 + 2, :],
                     ps[bk][:, :].rearrange("p (a b) -> p a b", a=2), [('ps', bk)], [('AB', tb, gp * 2), ('AB', tb, gp * 2 + 1)])
        dC = self.dftC.rearrange("(tb p) n -> p tb n", p=128)
        dS = self.dftS.rearrange("(tb p) n -> p tb n", p=128)
        for tq in range(8):
            qs = slice(tq * 256, (tq + 1) * 256)
            b.dma('sp', Ctab, dC[:, :, qs], (), ['Ctab'])
            b.dma('sp', Stab, dS[:, :, qs], (), ['Stab'])
            for g in range(4):
                for tb in range(NB):
                    b.mm(ps[g][:, 0:256], AB[:, tb, g, 0:128], Ctab[:, tb, :], tb == 0, False,
                         [('AB', tb, g), 'Ctab'], [('ps', g)])
            for g in range(4):
                for tb in range(NB):
                    b.mm(ps[g][:, 0:256], AB[:, tb, g, 128:256], Stab[:, tb, :], False, tb == NB - 1,
                         [('AB', tb, g), 'Stab'], [('ps', g)])
            for g in range(4):
                b.cp('act' if g % 2 == 0 else 'dve', ufT[:, g, qs], ps[g][:, 0:256], [('ps', g)],
                     [(('ufT', g), tq // 2)])
        b.emit()

        if self.dbg_stop <= 3:
            return self.post_ln_block(l)
        b = self.newblk()
        ar = Arena(A, PERSIST + OUTS)
        q0 = ar.bf([2, 2048])
        k0 = ar.bf([2, 2048])
        kh0 = ar.bf([16, 256])
        logg = ar.bf([16, 512])
        wa = ar.bf([512])
        ba = ar.bf([512])
        tmpa = [ar.f32(512) for _ in range(2)]
        tmpb = [ar.f32(512) for _ in range(2)]
        qd = [q0, qT]
        kd = [k0, kT]
        khd = [kh0, k_tm]
        b.add('dve', lambda e: e.memset(wa, 0.0), (), ['wa'])
        b.add('dve', lambda e: e.memset(ba, 0.0), (), ['ba'])
        b.dma('pool', wa[0:16, 0:256], self.w_a2[i, 0], (), ['wa'])
        b.dma('pool', wa[32:48, 256:512], self.w_a2[i, 1], (), ['wa'])
        b.dma('pool', ba[0:1, :], self.b_a[i].rearrange("(o z) e -> o (z e)", o=1), (), ['ba'])
        one_col = self.ones[:, 0:1]
        E0 = self.cb(0)
        for tb in range(NB):
            bk = b.bank()
            tsl = slice(tb * 128, (tb + 1) * 128)
            b.mm(ps[bk][:, :], E0, ba, True, False, ['cstb', 'ba'], [('ps', bk)])
            b.mm(ps[bk][:, :], alrT[:, tsl], wa, False, True, ['wa'], [('ps', bk)])
            tp = tmpa[tb % 2]
            tk = ('tmpa', tb % 2)
            b.act(tp, ps[bk][:, :], AF.Exp, [('ps', bk)], [tk], scale=-1.0)
            b.act(tp, tp, AF.Ln, [tk], [tk], bias=one_col)
            b.ts('dve', logg[:, tb, :], tp, -1.0 / 16.0, None, ALU.mult, None, [tk], [('logg', tb)])
        itc = 0
        for d in range(2 if self.dbg_sub >= 2 else 0):
            tri = self.cb(1 + d)
            for c in range(2):
                for t in range(NT):
                    sl = slice(t * TT, (t + 1) * TT)
                    bk = b.bank()
                    for q in range(4):
                        tb = t * 4 + q
                        b.mm(ps[bk][:, q * 128:(q + 1) * 128], logg[:, tb, d * 256 + c * 128:d * 256 + (c + 1) * 128],
                             tri, True, True, [('logg', tb), 'cstb'], [('ps', bk)])
                    pos = 63 if d == 0 else 0
                    e0 = (d * 2 + c) * 32 + t * 8
                    b.act(elast[:, e0:e0 + 8], ps[bk][:, :].rearrange("p (a b) -> p a b", a=8)[:, :, pos], AF.Exp,
                          [('ps', bk)], [('elast', d, c, t)])
                    ta = tmpa[itc % 2]
                    tbq = tmpb[itc % 2]
                    ka, kb = ('tmpa', itc % 2), ('tmpb', itc % 2)
                    itc += 1
                    b.act(ta, ps[bk][:, :], AF.Exp, [('ps', bk)], [ka])
                    b.act(tbq, ps[bk][:, :], AF.Exp, [('ps', bk)], [kb], scale=-1.0)
                    b.tt('dve', qd[d][:, c, sl], qT[:, c, sl], ta, ALU.mult, [('qT', c, t), ka], [('qd', d, c, t), ('qT', c, t)] if d == 1 else [('qd', d, c, t)])
                    b.tt('pool', kd[d][:, c, sl], kT[:, c, sl], tbq, ALU.mult, [('kT', c, t), kb], [('kd', d, c, t), ('kT', c, t)] if d == 1 else [('kd', d, c, t)])
            sm = self.cb(3 + d)
            for tb2 in range(NB // 2 if self.dbg_sub >= 3 else 0):
                bk = b.bank()
                for q in range(2):
                    tb = tb2 * 2 + q
                    b.mm(ps[bk][:, q * 256:(q + 1) * 256], sm, logg[:, tb, d * 256:(d + 1) * 256], True, True,
                         [('logg', tb), 'cstb'], [('ps', bk)])
                ta = tmpa[itc % 2]
                ka = ('tmpa', itc % 2)
                itc += 1
                b.act(ta, ps[bk][:, :], AF.Exp, [('ps', bk)], [ka])
                b.tt('dve', khd[d][:, tb2 * 2:tb2 * 2 + 2, :], k_tm[:, tb2 * 2:tb2 * 2 + 2, :],
                     ta.rearrange("p (a b) -> p a b", a=2), ALU.mult, [('k_tm', tb2), ka],
                     [('khd', d, tb2), ('k_tm', tb2)] if d == 1 else [('khd', d, tb2)])
        b.emit()

        if self.dbg_stop <= 4:
            return self.post_ln_block(l)
        b = self.newblk()
        Sbf = Arena(A, PERSIST + 25600).bf([128, 128])
        Mst = Arena(A, PERSIST + 12288).f32v([4, 256])
        for s4 in range(4):
            b.add('dve', lambda e, s4=s4: e.memset(Mst[:, s4, :], 0.0), (), [('M', s4)])
        for d in range(2):
            for p in range(2):
                s4 = d * 2 + p
                b.dma('sp', Mst[0:64, s4, 0:128], self.st0[i, d, 2 * p], (), [('M', s4)])
                b.dma('sp', Mst[64:128, s4, 128:256], self.st0[i, d, 2 * p + 1], (), [('M', s4)])
        for step in range(32):
            for d in range(2):
                c = step if d == 0 else 31 - step
                tb = c // 2
                r0 = (c % 2) * 64
                for p in range(2):
                    s4 = d * 2 + p
                    M = Mst[:, s4, :]
                    sidx = s4 * 32 + c
                    b.cp('act', Sbf[0:64, sidx, :], M[0:64, 0:128], [('M', s4)], [('Sbf', sidx)])
                    b.cp('pool', Sbf[64:128, sidx, :], M[64:128, 128:256], [('M', s4)], [('Sbf', sidx)])
                    bk = b.bank()
                    b.mm(ps[bk][:, 0:256], khd[d][r0:r0 + 64, tb, p * 128:(p + 1) * 128],
                         v_tm[r0:r0 + 64, tb, p * 256:(p + 1) * 256], True, True, [], [('ps', bk)])
                    ei = (d * 2 + p) * 32 + c
                    b.stt(M, M, elast[:, ei:ei + 1], ps[bk][:, 0:256], ALU.mult, ALU.add, [('M', s4), ('ps', bk)],
                          [('M', s4)])
                    boundary = (c % 4 == 3) if d == 0 else (c % 4 == 0)
                    if boundary:
                        n = c // 4
                        b.dma('sp', self.ns[i, d, n, 2 * p], M[0:64, 0:128], [('M', s4)], [])
                        b.dma('sp', self.ns[i, d, n, 2 * p + 1], M[64:128, 128:256], [('M', s4)], [])
                        b.ts('dve', M, M, self.keep_sb[:, n:n + 1], None, ALU.mult, None, [('M', s4)], [('M', s4)])
        b.emit()

        if self.dbg_stop <= 5:
            return self.post_ln_block(l)
        b = self.newblk()
        atm = Arena(A, PERSIST + 12288)
        attm = [atm.bf([256]) for _ in range(2)]
        sqb = atm.f32(256)
        isb = atm.f32(256)
        oT = [Arena(A, PERSIST + 13312).bf([2, 2048]), Arena(A, PERSIST + 23552).bf([2, 2048])]
        bm2 = self.cc(C_CSTB, 512).bitcast(BF16)[:, 5 * 128:7 * 128]
        gcol = self.cc(C_GNG + i, 1)
        ita = 0
        for h in range(4):
            p = h // 2
            r0 = (h % 2) * 64
            for t2 in range(8):
                sl = slice(t2 * 256, (t2 + 1) * 256)
                bo = t2 % 2
                bi = 2 + t2 % 2
                for q in range(2):
                    tb = t2 * 2 + q
                    tsl = slice(tb * 128, (tb + 1) * 128)
                    ba_ = 4 + ita % 2
                    am = attm[ita % 2]
                    ak = ('attm', ita % 2)
                    ita += 1
                    for d in range(2):
                        b.mm(ps[ba_][:, d * 128:(d + 1) * 128], kd[d][r0:r0 + 64, p, tsl], qd[d][r0:r0 + 64, p, tsl],
                             True, True, [], [('ps', ba_)])
                    b.tt('dve', am, ps[ba_][:, 0:256], bm2, ALU.mult, [('ps', ba_)], [ak])
                    oc = ps[bo][:, q * 128:(q + 1) * 128]
                    b.mm(oc, v_tm[:, tb, h * 128:(h + 1) * 128], am[:, 0:128], True, False, [ak], [('ps', bo)])
                    b.mm(oc, v_tm[:, tb, h * 128:(h + 1) * 128], am[:, 128:256], False, True, [ak], [('ps', bo)])
                    for cc in range(2):
                        c = tb * 2 + cc
                        for d in range(2):
                            sidx = (d * 2 + p) * 32 + c
                            b.mm(ps[bi][:, q * 128 + cc * 64:q * 128 + (cc + 1) * 64], Sbf[r0:r0 + 64, sidx, :],
                                 qd[d][r0:r0 + 64, p, c * 64:(c + 1) * 64], d == 0, d == 1, [], [('ps', bi)])
                b.cp('act', isb, ps[bi][:, 0:256], [('ps', bi)], ['isb'])
                b.tt('dve', isb, ps[bo][:, 0:256], isb, ALU.add, [('ps', bo), 'isb'], ['isb'])
                b.act(sqb, isb, AF.Square, ['isb'], ['sqb'])
                b.mm(ps[6][:, 0:256], ones, sqb, True, True, ['sqb'], [('ps', 6)])
                b.act(sqb, ps[6][:, 0:256], AF.Sqrt, [('ps', 6)], ['sqb'], bias=self.eps6, scale=1.0 / 128)
                b.recip(sqb, sqb, ['sqb'], ['sqb'])
                b.tt('dve', isb, isb, sqb, ALU.mult, ['isb', 'sqb'], ['isb'])
                b.stt(oT[p][:, h % 2, sl], isb, gcol, sgT[:, h, sl], ALU.mult, ALU.mult, ['isb'], [('oT', h, t2)])
        b.emit()

        if self.dbg_stop <= 6:
            return self.post_ln_block(l)
        b = self.newblk()
        Wo = Arena(A, PERSIST + 25600).bf([8, 1024])
        for kc in range(KC):
            b.dma('pool', Wo[:, kc, :], self.w_oe[i, kc * 128:(kc + 1) * 128, :], (), [('Wo', kc)])
        for m in range(KC):
            for t in range(NT):
                sl = slice(t * TT, (t + 1) * TT)
                bk = b.bank()
                for kk in range(8):
                    rhs = ufT[:, kk, sl] if kk < 4 else oT[(kk - 4) // 2][:, (kk - 4) % 2, sl]
                    b.mm(ps[bk][:, :], Wo[:, kk, m * 128:(m + 1) * 128], rhs, kk == 0, kk == 7, [('Wo', kk)], [('ps', bk)])
                xk = ('xT', m, t)
                b.stt(self.xT[:, m, sl], ps[bk][:, :], self.modcol(l, 2, m), self.xT[:, m, sl], ALU.mult, ALU.add,
                      [('ps', bk), xk], [xk])
        b.emit()
        self.post_ln_block(l)


    OUTS = 19456

    def pre_ln_block(self, l, base):
        b = self.newblk()
        ar = Arena(self.arena, PERSIST + base)
        hT = ar.bf([8, 2048])
        self.ln(b, ar, range(NT), lambda kc: self.modcol(l, 1, kc), lambda kc: self.modcol(l, 0, kc), self.eps5,
                lambda kc, t: hT[:, kc, t * TT:(t + 1) * TT], lambda kc, t: [('hT', kc, t)], prescale=ALPHA)
        b.emit()

    def post_ln_block(self, l):
        b = self.newblk()
        ar = Arena(self.arena, PERSIST)
        self.ln(b, ar, range(NT), lambda kc: self.lncol(l, 0, 0, kc), lambda kc: self.lncol(l, 0, 1, kc), self.eps5,
                lambda kc, t: self.xT[:, kc, t * TT:(t + 1) * TT], lambda kc, t: [('xT', kc, t)])
        b.emit()

    def odd_layer(self, l):
        i = l // 2
        self.pre_ln_block(l, 0)
        b = self.newblk()
        ps = self.ps
        ar = Arena(self.arena, PERSIST)
        hT = ar.bf([8, 2048])
        qT = ar.bf([8, 2048])
        W = ar.bf([8, 1536])
        kT = ar.bf([2, 2304])
        vS = ar.bf([18, 256])
        rC = ar.f32(512)
        rS = ar.f32(512)
        y32 = ar.f32(512)
        sq32 = ar.f32(512)
        rs = ar.f32(512)
        t1 = ar.f32(512)
        pT = [ar.bf([512]) for _ in range(3)]
        rden = ar.f32(512)
        v32 = [ar.f32(256) for _ in range(2)]
        k32 = [ar.f32(256) for _ in range(2)]
        cks = ar.f32v([2, 256])
        kbc = ar.f32(128)
        ssq = ar.f32(2)
        ident, ones, onesb = self.ident, self.ones, self.cb(7)
        qg = self.cc(C_QNG + i, 1)
        kg = self.cc(C_KNG + i, 1)
        for kc in range(KC):
            b.dma('pool', W[:, kc, :], self.w_qkv[i, kc * 128:(kc + 1) * 128, :], (), [('W', kc)])
        b.dma('sp', cks, self.ck[i].rearrange("(sb p) f -> p sb f", p=128), (), ['cks'])
        b.dma('pool', vS[:, 0:2, :], self.cv[i].rearrange("(sb p) f -> p sb f", p=128), (), [('vS', 0), ('vS', 1)])
        b.dma('sp', kbc, self.kng_bc[:, i * 128:(i + 1) * 128], (), ['kbc'])
        for sbi in range(2):
            bk = 7
            for kv in range(2):
                b.tr(ps[bk][:, kv * 128:(kv + 1) * 128], cks[:, sbi, kv * 128:(kv + 1) * 128], ident,
                     ['cks', 'cst'], [('ps', bk)])
            for kv in range(2):
                b.cp('act', kT[:, kv, sbi * 128:(sbi + 1) * 128], ps[bk][:, kv * 128:(kv + 1) * 128],
                     [('ps', bk)], [('kT', kv, sbi)])
        for tb in range(NB):
            bk = 4 + tb % 4
            tsl = slice(tb * 128, (tb + 1) * 128)
            for kc in range(KC):
                b.mm(ps[bk][:, 0:256], hT[:, kc, tsl], W[:, kc, 1280:1536], kc == 0, kc == KC - 1,
                     [('hT', kc, tb // 4), ('W', kc)], [('ps', bk)])
            for kc in range(KC):
                b.mm(ps[bk][:, 256:512], hT[:, kc, tsl], W[:, kc, 1024:1280], kc == 0, kc == KC - 1,
                     [('hT', kc, tb // 4), ('W', kc)], [('ps', bk)])
            v = v32[tb % 2]
            k = k32[tb % 2]
            b.cp('act', v, ps[bk][:, 0:256], [('ps', bk)], [('v32', tb % 2)])
            b.dma('sp', self.nv[i, tsl, :], v, [('v32', tb % 2)], [])
            b.cp('pool', vS[:, 2 + tb, :], v, [('v32', tb % 2)], [('vS', 2 + tb)])
            for kv in range(2):
                b.act(k[:, kv * 128:(kv + 1) * 128], ps[bk][:, 256 + kv * 128:256 + (kv + 1) * 128], AF.Square,
                      [('ps', bk)], [('k32', tb % 2), ('ssq', kv)], accum_out=ssq[:, kv:kv + 1])
            b.act(ssq, ssq, AF.Sqrt, [('ssq', 0), ('ssq', 1), 'cst'], [('ssq', 0), ('ssq', 1)], bias=self.eps6,
                  scale=1.0 / 128)
            b.recip(ssq, ssq, [('ssq', 0), ('ssq', 1)], [('ssq', 0), ('ssq', 1)])
            for kv in range(2):
                b.stt(k[:, kv * 128:(kv + 1) * 128], ps[bk][:, 256 + kv * 128:256 + (kv + 1) * 128],
                      ssq[:, kv:kv + 1], kbc, ALU.mult, ALU.mult, [('ps', bk), ('ssq', kv), 'kbc'], [('k32', tb % 2)])
            b.dma('sp', self.nk[i, tsl, :], k, [('k32', tb % 2)], [])
        for t in range(NT):
            sl = slice(t * TT, (t + 1) * TT)
            b.dma('sp', rC, self.ropeC[:, sl], (), ['rC'])
            b.dma('sp', rS, self.ropeS[:, sl], (), ['rS'])
            for hc in range(10):
                col0 = hc * 128 if hc < 8 else 1024 + (hc - 8) * 128
                gcol = qg if hc < 8 else kg
                bq = 4 + hc % 2
                for kc in range(KC):
                    b.mm(ps[bq][:, :], W[:, kc, col0:col0 + 128], hT[:, kc, sl], kc == 0, kc == KC - 1,
                         [('W', kc), ('hT', kc, t)], [('ps', bq)])
                b.act(y32, ps[bq][:, :], AF.Identity, [('ps', bq), 'small'], ['y32'], scale=gcol)
                b.act(sq32, ps[bq][:, :], AF.Square, [('ps', bq)], ['sq32'])
                b.mm(ps[6][:, :], ones, sq32, True, True, ['cst', 'sq32'], [('ps', 6)])
                b.mm(ps[7][:, :], self.RT, y32, True, True, ['cst', 'y32'], [('ps', 7)])
                b.act(rs, ps[6][:, :], AF.Sqrt, [('ps', 6), 'cst'], ['rs'], bias=self.eps6, scale=1.0 / 128)
                b.recip(rs, rs, ['rs'], ['rs'])
                b.tt('pool', t1, y32, rC, ALU.mult, ['y32', 'rC'], ['t1'])
                b.tt('dve', sq32, ps[7][:, :], rS, ALU.mult, [('ps', 7), 'rS'], ['sq32'])
                b.tt('pool', t1, t1, sq32, ALU.add, ['t1', 'sq32'], ['t1'])
                if hc < 8:
                    b.tt('dve', qT[:, hc, sl], t1, rs, ALU.mult, ['t1', 'rs'], [('qT', hc, t)])
                else:
                    kv = hc - 8
                    b.tt('dve', kT[:, kv, 256 + t * TT:256 + (t + 1) * TT], t1, rs, ALU.mult, ['t1', 'rs'],
                         [('kT', kv, 2 + t * 4 + q) for q in range(4)])
        scale = 128.0 ** -0.5
        iters = [(h, qp, sbi) for h in range(8) for qp in range(4) for sbi in range(18)]

        def qk(n):
            h, qp, sbi = iters[n]
            kv = h // 4
            q0 = qp * TT
            bs = 4 + n % 3
            for hh in range(2):
                b.mm(ps[bs][:, hh * 256:(hh + 1) * 256], kT[:, kv, sbi * 128:(sbi + 1) * 128],
                     qT[:, h, q0 + hh * 256:q0 + (hh + 1) * 256], True, True,
                     [('kT', kv, sbi), ('qT', h, qp)], [('ps', bs)])
            p = pT[n % 3]
            pk = ('pT', n % 3)
            for hh in range(2):
                c = sbi * 8 + qp * 2 + hh
                b.act(p[:, hh * 256:(hh + 1) * 256], ps[bs][:, hh * 256:(hh + 1) * 256], AF.Exp,
                      [('ps', bs), 'small'], [pk], bias=self.attb_sb[:, c:c + 1], scale=scale)

        def pv(n):
            h, qp, sbi = iters[n]
            kv = h // 4
            q0 = qp * TT
            grp = h * 4 + qp
            bo = grp % 2
            bd = 2 + grp % 2
            p = pT[n % 3]
            pk = ('pT', n % 3)
            b.mm(ps[bo][:, :], vS[:, sbi, kv * 128:(kv + 1) * 128], p, sbi == 0, sbi == 17,
                 [('vS', sbi), pk], [('ps', bo)])
            b.mm(ps[bd][:, :], onesb, p, sbi == 0, sbi == 17, ['cstb', pk], [('ps', bd)])
            if sbi == 17:
                b.recip(rden, ps[bd][:, :], [('ps', bd)], ['rden'])
                b.tt('dve', hT[:, h, q0:q0 + TT], ps[bo][:, :], rden, ALU.mult, [('ps', bo), 'rden'], [('hT', h, qp)])

        qk(0)
        for n in range(len(iters)):
            if n + 1 < len(iters):
                qk(n + 1)
            pv(n)
        for kc in range(KC):
            b.dma('pool', W[:, kc, 0:1024], self.w_o[i, kc * 128:(kc + 1) * 128, :], (), [('W', kc)])
        for m in range(KC):
            for t in range(NT):
                sl = slice(t * TT, (t + 1) * TT)
                bk = 4 + (m * NT + t) % 4
                for hh in range(8):
                    b.mm(ps[bk][:, :], W[:, hh, m * 128:(m + 1) * 128], hT[:, hh, sl], hh == 0, hh == 7,
                         [('W', hh), ('hT', hh, t)], [('ps', bk)])
                xk = ('xT', m, t)
                b.stt(self.xT[:, m, sl], ps[bk][:, :], self.modcol(l, 2, m), self.xT[:, m, sl], ALU.mult, ALU.add,
                      [('ps', bk), xk, 'mods'], [xk])
        b.emit()
        self.post_ln_block(l)


def _bf(a):
    return np.ascontiguousarray(a.astype(ml_dtypes.bfloat16))


def _fm(v):
    v = np.asarray(v, np.float32)
    n = v.shape[-1] // 128
    lead = v.shape[:-1]
    r = v.reshape(lead + (n, 128))
    r = np.moveaxis(r, -1, 0)
    return np.ascontiguousarray(r.reshape(128, -1))


def _const_tables():
    cst = np.zeros((128, 512), np.float32)
    cst[:, 0:128] = np.eye(128, dtype=np.float32)
    RT = np.zeros((128, 128), np.float32)
    for i in range(64):
        RT[2 * i + 1, 2 * i] = -1.0
        RT[2 * i, 2 * i + 1] = 1.0
    cst[:, 128:256] = RT
    cst[:, 256:384] = 1.0
    cst[:, 384] = 1e-5
    cst[:, 385] = 1e-6
    cb = np.zeros((8, 128, 128), np.float32)
    cb[0] = 0.0
    cb[0][0, :] = 1.0
    j = np.arange(128)[:, None]
    i = np.arange(128)[None, :]
    same = (j // 64) == (i // 64)
    cb[1] = same & (j <= i)
    cb[2] = same & (j >= i)
    cb[3] = same & (j > i)
    cb[4] = same & (j < i)
    cb[5] = same & (j <= i)
    cb[6] = same & (j >= i)
    cb[7] = 1.0
    cstb = _bf(np.concatenate(list(cb), axis=1))
    c = np.arange(128)
    ang = 2.0 * np.pi * np.outer(c, c) / 128.0
    cs128 = _bf(np.concatenate([np.cos(ang), np.sin(ang)], axis=1) / np.sqrt(128.0))
    return cst, cstb, cs128


def _dft_tables(tseq):
    t = np.arange(T)
    blk = t // tseq
    loc = (t % tseq).astype(np.float64)
    ang = 2.0 * np.pi * np.outer(loc, loc) / tseq
    same = (blk[:, None] == blk[None, :])
    Cm = np.where(same, np.cos(ang), 0.0) / np.sqrt(tseq)
    Sm = np.where(same, -np.sin(ang), 0.0) / np.sqrt(tseq)
    return _bf(Cm.astype(np.float32)), _bf(Sm.astype(np.float32))


def _rope_tables(latent):
    if not latent:
        return np.ones((128, T), np.float32), np.zeros((128, T), np.float32)
    t = np.arange(T)
    row = (t // 64).astype(np.float32)
    col = (t % 64).astype(np.float32)
    freqs = (10000.0 ** (-np.arange(32, dtype=np.float32) / 32)).astype(np.float32)
    ang = np.concatenate([row[:, None] * freqs, col[:, None] * freqs], axis=-1)
    cos = np.cos(ang).astype(np.float32)
    sin = np.sin(ang).astype(np.float32)
    C = np.repeat(cos.T, 2, axis=0)
    S = np.repeat(sin.T, 2, axis=0)
    return np.ascontiguousarray(C), np.ascontiguousarray(S)


def _attb(latent):
    a = np.zeros((128, 144), np.float32)
    if not latent:
        for sb in range(18):
            for qt in range(8):
                ok = sb >= 2 and ((sb - 2) // 2 == qt)
                a[:, sb * 8 + qt] = 0.0 if ok else NEG_BIG
    return a


def prep_inputs(inp):
    g = lambda k: np.asarray(inp[k], np.float32)
    cst, cstb, cs128 = _const_tables()
    w_in = g('w_in_even')
    w_in_pad = np.zeros((2, D, 2176), np.float32)
    w_in_pad[:, :, 0:2048] = w_in[:, :, 0:2048]
    w_in_pad[:, :, 2048:2064] = w_in[:, :, 2048:2064]
    w_in_pad[:, :, 2080:2096] = w_in[:, :, 2064:2080]
    ln_g, ln_b = g('ln_g'), g('ln_b')
    lnT = _fm(np.stack([ln_g, ln_b], axis=2))
    shared = dict(
        cst=cst, cstb=cstb, cs128=cs128,
        w_mod=g('w_mod'), b_modT=_fm(g('b_mod')), lnT=lnT,
        w_in=w_in_pad, w_a2=g('w_a2'), b_a=g('b_a'), gng=np.ascontiguousarray(g('gla_norm_g').T),
        w_oe=g('w_out_even'), w_qkv=g('w_qkv'), qng=np.ascontiguousarray(g('q_norm_g').T),
        kng=np.ascontiguousarray(g('k_norm_g').T),
        kng_bc=np.ascontiguousarray(np.broadcast_to(g('k_norm_g').reshape(1, 256), (128, 256))),
        w_o=g('w_o'), w_r=g('w_router'), b_r=g('b_router'), w_gu=g('w_gate_up'),
        b_guT=_fm(g('b_gate_up')), w_d=g('w_down'), b_d=g('b_down'),
    )
    lat = dict(zip(('ropeC', 'ropeS'), _rope_tables(True)))
    lat['attb'] = _attb(True)
    lat['keep'] = np.ones((128, 8), np.float32)
    lat['dftC'], lat['dftS'] = _dft_tables(2048)
    pro = dict(zip(('ropeC', 'ropeS'), _rope_tables(False)))
    pro['attb'] = _attb(False)
    pro['keep'] = np.zeros((128, 8), np.float32)
    pro['dftC'], pro['dftS'] = _dft_tables(256)
    xs, xp = g('x_sample'), g('x_prompt')
    maps = []
    for core in range(8):
        m = dict(shared)
        if core < 4:
            m.update(lat)
            m['x'] = np.ascontiguousarray(xs[core])
            m['cT'] = _fm(g('c')[core])
            m['st0'] = np.ascontiguousarray(g('state_gla')[core])
            m['ck'] = np.ascontiguousarray(g('cache_k')[core].reshape(2, 256, 256))
            m['cv'] = np.ascontiguousarray(g('cache_v')[core].reshape(2, 256, 256))
        else:
            grp = (core - 4) % 2
            m.update(pro)
            m['x'] = np.ascontiguousarray(xp[grp * 8:(grp + 1) * 8].reshape(T, D))
            m['cT'] = _fm(g('c_ctx'))
            m['st0'] = np.zeros((2, 2, 4, 64, 128), np.float32)
            m['ck'] = np.zeros((2, 256, 256), np.float32)
            m['cv'] = np.zeros((2, 256, 256), np.float32)
        maps.append(m)
    return maps


_NC_CACHE = {}


def run(inputs, depth=DEPTH, mixers=True, cores=8):
    key = (depth, mixers)
    if key not in _NC_CACHE:
        _NC_CACHE[key] = Builder(depth, mixers).build()
    nc = _NC_CACHE[key]
    maps = prep_inputs(inputs)[:cores]
    res = run_bass_kernel_spmd(nc, maps, core_ids=list(range(cores)))
    return res.results


def kernel(**inputs):
    r = run(inputs)
    y_sample = np.stack([r[c]['y'] for c in range(4)], axis=0)
    y_prompt = np.concatenate([r[4 + gI]['y'].reshape(8, 256, D) for gI in range(2)], axis=0)
    ns = np.concatenate([np.transpose(r[4 + gI]['ns'], (2, 0, 1, 3, 4, 5)) for gI in range(2)], axis=0)
    nk = np.concatenate([np.transpose(r[4 + gI]['nk'].reshape(2, 8, 256, 2, 128), (1, 0, 2, 3, 4)) for gI in range(2)], axis=0)
    nv = np.concatenate([np.transpose(r[4 + gI]['nv'].reshape(2, 8, 256, 2, 128), (1, 0, 2, 3, 4)) for gI in range(2)], axis=0)
    return (np.ascontiguousarray(y_prompt, np.float32), np.ascontiguousarray(y_sample, np.float32),
            np.ascontiguousarray(ns, np.float32), np.ascontiguousarray(nk, np.float32),
            np.ascontiguousarray(nv, np.float32))
```

```python
import numpy as np
import ml_dtypes
import concourse.bass as bass
import concourse.mybir as mybir
from concourse.bass_utils import run_bass_kernel_spmd

F32 = mybir.dt.float32
BF16 = mybir.dt.bfloat16
AF = mybir.ActivationFunctionType
ALU = mybir.AluOpType
AX = mybir.AxisListType

D = 1024
KC = 8
T = 2048
TT = 512
NT = 4
NB = 16
DEPTH = 4
NE = 32
ALPHA = (2.0 * DEPTH) ** 0.25
NEG_BIG = -30000.0
ARENA_COLS = 52224
SAME_SYNC = True
NDS = 24

ENGS = ('pe', 'act', 'dve', 'pool', 'sp')
BLOCK_NAME = {'pe': 'tensor', 'act': 'scalar', 'dve': 'vector', 'pool': 'gpsimd', 'sp': 'sync'}


class Op:
    __slots__ = ('eng', 'fn', 'deps', 'is_dma', 'sem', 'val', 'prev')

    def __init__(self, eng, fn, deps, is_dma):
        self.eng = eng
        self.fn = fn
        self.deps = deps
        self.is_dma = is_dma
        self.sem = None
        self.val = None
        self.prev = 0


class Sync:
    def __init__(self, nc, stack):
        self.nc = nc
        self.eng_sem = {e: stack.enter_context(nc.semaphore('s_' + e)) for e in ('pe', 'act', 'dve', 'pool')}
        self.eng_cnt = {e: 0 for e in self.eng_sem}
        self.dma_sems = {q: [stack.enter_context(nc.semaphore('d_%s%d' % (q, i))) for i in range(NDS)]
                         for q in ('sp', 'pool')}
        self.dma_cnt = {q: [0] * NDS for q in ('sp', 'pool')}
        self.dma_rr = {q: 0 for q in ('sp', 'pool')}
        self.final_waits = []


class Blk:
    def __init__(self, nc, sync, ps):
        self.nc = nc
        self.sync = sync
        self.ops = []
        self.lw = {}
        self.rd = {}
        self.ps = ps
        self.bank_rr = 0

    def bank(self):
        b = self.bank_rr % 8
        self.bank_rr += 1
        return b

    def add(self, eng, fn, rd=(), wr=(), is_dma=False):
        idx = len(self.ops)
        deps = set()
        for k in rd:
            w = self.lw.get(k)
            if w is not None:
                deps.add(w)
        for k in wr:
            w = self.lw.get(k)
            if w is not None:
                deps.add(w)
            for r in self.rd.get(k, ()):
                deps.add(r)
        for k in rd:
            self.rd.setdefault(k, []).append(idx)
        for k in wr:
            self.lw[k] = idx
            self.rd[k] = []
        last = {}
        keep = []
        for dd in deps:
            od = self.ops[dd]
            if od.is_dma:
                keep.append(dd)
            elif last.get(od.eng, -1) < dd:
                last[od.eng] = dd
        keep.extend(last.values())
        self.ops.append(Op(eng, fn, sorted(keep), is_dma))
        return idx

    def mm(self, out, lhsT, rhs, start, stop, rd, wr):
        self.add('pe', lambda e: e.matmul(out, lhsT=lhsT, rhs=rhs, start=start, stop=stop), rd, wr)

    def tr(self, out, in_, ident, rd, wr):
        self.add('pe', lambda e: e.transpose(out, in_, ident), rd, wr)

    def act(self, out, in_, func, rd, wr, bias=None, scale=None, accum_out=None):
        kw = {}
        if bias is not None:
            kw['bias'] = bias
        if scale is not None:
            kw['scale'] = scale
        if accum_out is not None:
            kw['accum_out'] = accum_out
        self.add('act', lambda e: e.activation(out=out, in_=in_, func=func, **kw), rd, wr)

    def ts(self, eng, out, in0, s1, s2, op0, op1, rd, wr):
        if op1 is None:
            self.add(eng, lambda e: e.tensor_scalar(out=out, in0=in0, scalar1=s1, scalar2=None, op0=op0), rd, wr)
        else:
            self.add(eng, lambda e: e.tensor_scalar(out=out, in0=in0, scalar1=s1, scalar2=s2, op0=op0, op1=op1), rd, wr)

    def tt(self, eng, out, in0, in1, op, rd, wr):
        self.add(eng, lambda e: e.tensor_tensor(out=out, in0=in0, in1=in1, op=op), rd, wr)

    def stt(self, out, in0, scalar, in1, op0, op1, rd, wr):
        self.add('dve', lambda e: e.scalar_tensor_tensor(out=out, in0=in0, scalar=scalar, in1=in1, op0=op0, op1=op1), rd, wr)

    def cp(self, eng, out, in_, rd, wr):
        if eng == 'act':
            self.add('act', lambda e: e.activation(out=out, in_=in_, func=AF.Copy), rd, wr)
        else:
            self.add(eng, lambda e: e.tensor_copy(out=out, in_=in_), rd, wr)

    def recip(self, out, in_, rd, wr):
        self.add('dve', lambda e: e.reciprocal(out=out, in_=in_), rd, wr)

    def dma(self, q, out, in_, rd, wr):
        return self.add(q, lambda e: e.dma_start(out=out, in_=in_), rd, wr, is_dma=True)

    def emit(self, final=False):
        ops = self.ops
        sy = self.sync
        n = len(ops)
        signal = [False] * n
        for o in ops:
            for d in o.deps:
                od = ops[d]
                if od.is_dma:
                    continue
                if od.eng != o.eng or (SAME_SYNC and o.eng != 'pe'):
                    signal[d] = True
        for i, o in enumerate(ops):
            if o.is_dma:
                q = o.eng
                k = sy.dma_rr[q] % NDS
                sy.dma_rr[q] += 1
                o.sem = sy.dma_sems[q][k]
                o.prev = sy.dma_cnt[q][k]
                sy.dma_cnt[q][k] += 16
                o.val = sy.dma_cnt[q][k]
            elif signal[i]:
                sy.eng_cnt[o.eng] += 1
                o.val = sy.eng_cnt[o.eng]
                o.sem = sy.eng_sem[o.eng]
        out_dmas = [o for o in ops if o.is_dma and getattr(o, 'is_out', False)]
        with self.nc.Block() as block:
            for eng in ENGS:
                my = [o for o in ops if o.eng == eng]
                if not my and not (final and eng == 'sp'):
                    continue

                def body(e, eng=eng, my=my):
                    waited = {}

                    def w(sem, val):
                        k = id(sem)
                        if waited.get(k, 0) < val:
                            e.wait_ge(sem, val)
                            waited[k] = val
                    for o in my:
                        if o.is_dma and o.prev > 0:
                            w(o.sem, o.prev)
                        for d in o.deps:
                            od = ops[d]
                            if od.is_dma:
                                w(od.sem, od.val)
                            elif od.eng != eng or (SAME_SYNC and eng != 'pe'):
                                w(od.sem, od.val)
                        ins = o.fn(e)
                        if o.is_dma:
                            ins.then_inc(o.sem, 16)
                        elif o.val is not None:
                            ins.then_inc(o.sem, 1)
                    if eng == 'sp' and final:
                        for q in ('sp', 'pool'):
                            for k in range(NDS):
                                if sy.dma_cnt[q][k] > 0:
                                    w(sy.dma_sems[q][k], sy.dma_cnt[q][k])
                getattr(block, BLOCK_NAME[eng])(body)


class Arena:
    def __init__(self, ap, base):
        self.ap = ap
        self.off = base
        self.base = base

    def f32(self, ncols, shape=None):
        a = self.ap[:, self.off:self.off + ncols]
        self.off += ncols
        assert self.off <= ARENA_COLS, self.off
        return a

    def f32v(self, dims):
        n = int(np.prod(dims))
        a = self.f32(n)
        if len(dims) == 2:
            return a.rearrange("p (a b) -> p a b", a=dims[0])
        return a

    def bf(self, dims):
        n = int(np.prod(dims))
        assert n % 2 == 0
        a = self.f32(n // 2).bitcast(BF16)
        if len(dims) == 2:
            return a.rearrange("p (a b) -> p a b", a=dims[0])
        if len(dims) == 3:
            return a.rearrange("p (a b c) -> p a b c", a=dims[0], b=dims[1])
        return a


C_IDENT, C_RT, C_ONES, C_EPS5, C_EPS6 = 0, 128, 256, 384, 385
C_CSTB = 512
C_MOD = 1024
C_LN = 1216
C_QNG, C_KNG, C_GNG = 1344, 1346, 1348
C_ATTB = 1352
C_KEEP = 1496
C_SCB = 1504
C_CT = 1508
C_BMODT = 1520
PERSIST = 16384 + 2048


class Builder:
    def __init__(self, depth=DEPTH, mixers=True, ne_run=NE, moe=True, layers=None, dbg_stop=99, dbg_sub=99):
        self.dbg_stop = dbg_stop
        self.dbg_sub = dbg_sub
        self.layers = list(range(depth)) if layers is None else layers
        self.depth = depth
        self.mixers = mixers
        self.ne_run = ne_run
        self.moe = moe

    def dram(self, name, shape, dt=F32, out=False):
        return self.nc.dram_tensor(name, list(shape), dt, kind="ExternalOutput" if out else "ExternalInput").ap()

    def build(self):
        from contextlib import ExitStack
        nc = bass.Bass("TRN2", target_bir_lowering=False)
        self.nc = nc
        d = self.dram
        self.x = d("x", [T, D])
        self.cT = d("cT", [128, 8])
        self.st0 = d("st0", [2, 2, 4, 64, 128])
        self.ck = d("ck", [2, 256, 256])
        self.cv = d("cv", [2, 256, 256])
        self.ropeC = d("ropeC", [128, T])
        self.ropeS = d("ropeS", [128, T])
        self.attb = d("attb", [128, 144])
        self.keep = d("keep", [128, 8])
        self.dftC = d("dftC", [T, T], BF16)
        self.dftS = d("dftS", [T, T], BF16)
        self.cs128 = d("cs128", [128, 256], BF16)
        self.cst = d("cst", [128, 512])
        self.cstb = d("cstb", [128, 1024], BF16)
        self.w_mod = d("w_mod", [4, D, 6 * D])
        self.b_modT = d("b_modT", [128, 192])
        self.lnT = d("lnT", [128, 128])
        self.w_in = d("w_in", [2, D, 2176])
        self.w_a2 = d("w_a2", [2, 2, 16, 256])
        self.b_a = d("b_a", [2, 2, 256])
        self.gng = d("gng", [128, 2])
        self.w_oe = d("w_oe", [2, D, D])
        self.w_qkv = d("w_qkv", [2, D, 1536])
        self.qng = d("qng", [128, 2])
        self.kng = d("kng", [128, 2])
        self.kng_bc = d("kng_bc", [128, 256])
        self.w_o = d("w_o", [2, D, D])
        self.w_r = d("w_r", [4, D, NE])
        self.b_r = d("b_r", [4, NE])
        self.w_gu = d("w_gu", [4, NE, D, 2 * D] if self.moe else [1, 1, 128, 128])
        self.b_guT = d("b_guT", [128, 4 * NE * 16])
        self.w_d = d("w_d", [4, NE, D, D] if self.moe else [1, 1, 128, 128])
        self.b_d = d("b_d", [4, NE, D])
        self.y = d("y", [T, D], out=True)
        self.ns = d("ns", [2, 2, 8, 4, 64, 128], out=True)
        self.nk = d("nk", [2, T, 256], out=True)
        self.nv = d("nv", [2, T, 256], out=True)

        with ExitStack() as stack:
            arena = stack.enter_context(nc.sbuf_tensor("arena", [128, ARENA_COLS], F32))
            self.arena = arena[:, :]
            self.ps = [stack.enter_context(nc.psum_tensor("ps%d" % i, [128, 512], F32)) for i in range(8)]
            self.sync = Sync(nc, stack)
            A = self.arena
            self.xT = A[:, 0:16384].rearrange("p (a b) -> p a b", a=8)
            P0 = 16384
            self.cc = lambda off, n: A[:, P0 + off:P0 + off + n]
            self.ident = self.cc(C_IDENT, 128)
            self.RT = self.cc(C_RT, 128)
            self.ones = self.cc(C_ONES, 128)
            self.eps5 = self.cc(C_EPS5, 1)
            self.eps6 = self.cc(C_EPS6, 1)
            cb = self.cc(C_CSTB, 512).bitcast(BF16)
            self.cb = lambda i: cb[:, i * 128:(i + 1) * 128]
            self.mod = self.cc(C_MOD, 192)
            self.lnp = self.cc(C_LN, 128)
            self.attb_sb = self.cc(C_ATTB, 144)
            self.keep_sb = self.cc(C_KEEP, 8)

            self.prologue()
            for l in self.layers:
                if self.mixers:
                    if l % 2 == 0:
                        self.even_layer(l)
                    else:
                        self.odd_layer(l)
                else:
                    self.nomix_layer(l)
                for half in range(2):
                    if self.moe:
                        self.moe_router(l, half)
                        self.moe_experts(l, half)
            self.epilogue()
            self.counts = dict(self.sync.eng_cnt)
        return nc

    def newblk(self):
        return Blk(self.nc, self.sync, self.ps)

    def prologue(self):
        b = self.newblk()
        ps = self.ps
        A = self.arena
        P0 = 16384
        b.dma('sp', self.cc(0, 512), self.cst[:, :], (), ['cst'])
        b.dma('sp', self.cc(C_CSTB, 512).bitcast(BF16), self.cstb[:, :], (), ['cstb'])
        b.dma('sp', self.lnp, self.lnT[:, :], (), ['lnp'])
        b.dma('sp', self.cc(C_QNG, 2), self.qng[:, :], (), ['small'])
        b.dma('sp', self.cc(C_KNG, 2), self.kng[:, :], (), ['small'])
        b.dma('sp', self.cc(C_GNG, 2), self.gng[:, :], (), ['small'])
        b.dma('sp', self.attb_sb, self.attb[:, :], (), ['small'])
        b.dma('sp', self.keep_sb, self.keep[:, :], (), ['small'])
        b.dma('sp', self.cc(C_CT, 8), self.cT[:, :], (), ['cT'])
        b.dma('sp', self.cc(C_BMODT, 192), self.b_modT[:, :], (), ['bmodT'])
        ar = Arena(A, PERSIST)
        xs = [ar.f32(1024) for _ in range(2)]
        for tb in range(NB):
            s = xs[tb % 2]
            b.dma('sp', s, self.x[tb * 128:(tb + 1) * 128, :], (), [('xs', tb % 2)])
            for half in range(2):
                bk = b.bank()
                for c in range(4):
                    kc = half * 4 + c
                    b.tr(ps[bk][:, c * 128:(c + 1) * 128], s[:, kc * 128:(kc + 1) * 128], self.ident,
                         [('xs', tb % 2), 'cst'], [('ps', bk)])
                b.cp('act' if half == 0 else 'dve',
                     self.xT[:, half * 4:half * 4 + 4, tb * 128:(tb + 1) * 128],
                     ps[bk][:, :].rearrange("p (a b) -> p a b", a=4),
                     [('ps', bk)], [('xT', kc, tb // 4) for kc in range(half * 4, half * 4 + 4)])
        scb = self.cc(C_SCB, 4).bitcast(BF16)
        b.act(scb, self.cc(C_CT, 8), AF.Silu, ['cT'], ['scb'])
        wm = [ar.bf([8, 3072]) for _ in range(2)]
        bkM = b.bank()
        it = 0
        for l in self.layers[:1]:
            for half in range(2):
                w = wm[it % 2]
                for kc in range(KC):
                    b.dma('pool', w[:, kc, :], self.w_mod[l, kc * 128:(kc + 1) * 128, half * 3072:(half + 1) * 3072],
                          (), [('wm', it % 2, kc)])
                for oc in range(24):
                    col = half * 24 + oc
                    for kc in range(KC):
                        b.mm(ps[bkM][:, col:col + 1], w[:, kc, oc * 128:(oc + 1) * 128], scb[:, kc:kc + 1],
                             kc == 0, kc == KC - 1, [('wm', it % 2, kc), 'scb'], [('ps', bkM)])
                it += 1
            b.tt('dve', self.mod[:, l * 48:(l + 1) * 48], ps[bkM][:, 0:48], self.cc(C_BMODT + l * 48, 48), ALU.add,
                 [('ps', bkM), 'bmodT'], [('mod', l)])
            for s in (1, 4):
                sl = self.mod[:, l * 48 + s * 8:l * 48 + s * 8 + 8]
                b.ts('dve', sl, sl, 1.0, None, ALU.add, None, [('mod', l)], [('mod', l)])
        b.emit()

    def ln(self, b, ar, tiles, scale_of, shift_of, eps, out_of, out_keys_of, extra=None, prescale=None,
           src_keys=None):
        ps = self.ps
        sqb = [ar.f32(TT) for _ in range(2)]
        tmp = [ar.f32(TT) for _ in range(4)]
        mean = ar.f32(TT)
        msq = ar.f32(TT)
        rstd = ar.f32(TT)
        nmr = ar.f32(TT)
        for t in tiles:
            sl = slice(t * TT, (t + 1) * TT)
            bm = b.bank()
            bq = b.bank()
            for kc in range(KC):
                xk = ('xT', kc, t)
                sq = sqb[kc % 2]
                b.act(sq, self.xT[:, kc, sl], AF.Square, [xk], [('sqb', kc % 2)])
                b.mm(ps[bm][:, :], self.ones, self.xT[:, kc, sl], kc == 0, kc == KC - 1, [xk, 'cst'], [('ps', bm)])
                b.mm(ps[bq][:, :], self.ones, sq, kc == 0, kc == KC - 1, [('sqb', kc % 2), 'cst'], [('ps', bq)])
            b.act(mean, ps[bm][:, :], AF.Copy, [('ps', bm)], ['ln_mean'], scale=1.0 / D)
            b.tt('dve', msq, mean, mean, ALU.mult, ['ln_mean'], ['ln_msq'])
            b.stt(msq, ps[bq][:, :], 1.0 / D, msq, ALU.mult, ALU.subtract, [('ps', bq), 'ln_msq'], ['ln_msq'])
            b.act(msq, msq, AF.Sqrt, ['ln_msq', 'cst'], ['ln_msq'], bias=eps)
            b.recip(rstd, msq, ['ln_msq'], ['ln_rstd'])
            b.stt(nmr, mean, -1.0, rstd, ALU.mult, ALU.mult, ['ln_mean', 'ln_rstd'], ['ln_nmr'])
            for kc in range(KC):
                xk = ('xT', kc, t)
                tp = tmp[kc % 4]
                tk = ('lntmp', kc % 4)
                b.tt('dve', tp, self.xT[:, kc, sl], rstd, ALU.mult, [xk, 'ln_rstd'], [tk])
                b.tt('dve', tp, tp, nmr, ALU.add, [tk, 'ln_nmr'], [tk])
                b.act(out_of(kc, t), tp, AF.Identity, [tk, 'mods'], out_keys_of(kc, t),
                      bias=shift_of(kc), scale=scale_of(kc))
                if extra is not None:
                    eo, ek = extra(kc, t)
                    b.act(eo, tp, AF.Identity, [tk, 'mods'], ek, bias=shift_of(kc), scale=scale_of(kc))
                if prescale is not None:
                    b.act(self.xT[:, kc, sl], self.xT[:, kc, sl], AF.Copy, [xk], [xk], scale=float(prescale))

    def modcol(self, l, s, kc):
        i = l * 48 + s * 8 + kc
        return self.mod[:, i:i + 1]

    def lncol(self, l, which, gb, kc):
        i = ((l * 2 + which) * 2 + gb) * 8 + kc
        return self.lnp[:, i:i + 1]

    def nomix_layer(self, l):
        b = self.newblk()
        ar = Arena(self.arena, PERSIST)
        for kc in range(KC):
            for t in range(NT):
                sl = slice(t * TT, (t + 1) * TT)
                b.ts('pool', self.xT[:, kc, sl], self.xT[:, kc, sl], float(ALPHA), None, ALU.mult, None,
                     [('xT', kc, t)], [('xT', kc, t)])
        self.ln(b, ar, range(NT), lambda kc: self.lncol(l, 0, 0, kc), lambda kc: self.lncol(l, 0, 1, kc), self.eps5,
                lambda kc, t: self.xT[:, kc, t * TT:(t + 1) * TT], lambda kc, t: [('xT', kc, t)])
        b.emit()

    def moe_router(self, l, half):
        b = self.newblk()
        ps = self.ps
        ar = Arena(self.arena, PERSIST)
        self.hT = ar.bf([8, 1024])
        self.combT = ar.bf([1024])
        h32s = [ar.f32v([8, TT]) for _ in range(2)]
        wr = ar.f32v([8, NE])
        br = ar.f32(NE)
        lg2 = [ar.f32(NE) for _ in range(2)]
        m82 = [ar.f32(8) for _ in range(2)]
        nmax2 = [ar.f32(1) for _ in range(2)]
        ex2 = [ar.f32(NE) for _ in range(2)]
        mask2 = [ar.f32(NE) for _ in range(2)]
        ssum2 = [ar.f32(1) for _ in range(2)]
        comb2 = [ar.f32(NE) for _ in range(2)]
        nxt = None
        if l in self.layers and self.layers.index(l) + 1 < len(self.layers):
            nxt = self.layers[self.layers.index(l) + 1]
        wmq = [ar.bf([8, 768]) for _ in range(4)] if nxt is not None else None
        if nxt is not None:
            for q4 in range(4):
                q = half * 4 + q4
                for kc in range(KC):
                    b.dma('pool', wmq[q4][:, kc, :], self.w_mod[nxt, kc * 128:(kc + 1) * 128, q * 768:(q + 1) * 768],
                          (), [('wmq', q4, kc)])
        b.dma('sp', wr, self.w_r[l].rearrange("(kc p) n -> p kc n", p=128), (), ['wr'])
        b.dma('sp', br[0:1, :], self.b_r[l:l + 1, :], (), ['br'])
        tiles = [half * 2, half * 2 + 1]

        def out_of(kc, t):
            tl = t - half * 2
            return self.hT[:, kc, tl * TT:(tl + 1) * TT]

        lnar = Arena(self.arena, ar.off)
        first = True
        for t in tiles:
            tl = t - half * 2
            sub = Arena(self.arena, lnar.base)
            h32 = h32s[tl]
            self.ln(b, sub, [t], lambda kc: self.modcol(l, 4, kc), lambda kc: self.modcol(l, 3, kc), self.eps5,
                    out_of, lambda kc, t: [('hT', kc, t - half * 2)],
                    extra=lambda kc, t, h32=h32, tl=tl: (h32[:, kc, :], [('h32', kc, tl)]), prescale=ALPHA)
            for q in range(4):
                tbl = tl * 4 + q
                r2 = tbl % 2
                lg, m8, nmax, ex, mask, ssum, comb = lg2[r2], m82[r2], nmax2[r2], ex2[r2], mask2[r2], ssum2[r2], comb2[r2]
                K_ = lambda n: (n, r2)
                bk = b.bank()
                b.mm(ps[bk][:, 0:NE], self.ones[0:1, 0:128], br[0:1, :], True, False, ['cst', 'br'], [('ps', bk)])
                for kc in range(KC):
                    b.mm(ps[bk][:, 0:NE], h32[:, kc, q * 128:(q + 1) * 128], wr[:, kc, :], False, kc == KC - 1,
                         [('h32', kc, tl), 'wr'], [('ps', bk)])
                b.cp('act', lg, ps[bk][:, 0:NE], [('ps', bk)], [K_('lg')])
                b.add('dve', lambda e, m8=m8, lg=lg: e.max(out=m8, in_=lg), [K_('lg')], [K_('m8')])
                b.ts('dve', nmax, m8[:, 0:1], -1.0, None, ALU.mult, None, [K_('m8')], [K_('nmax')])
                b.act(ex, lg, AF.Exp, [K_('lg'), K_('nmax')], [K_('ex')], bias=nmax)
                b.ts('dve', mask, lg, m8[:, 3:4], None, ALU.is_ge, None, [K_('lg'), K_('m8')], [K_('mask')])
                b.tt('dve', ex, ex, mask, ALU.mult, [K_('ex'), K_('mask')], [K_('ex')])
                b.add('dve', lambda e, ssum=ssum, ex=ex: e.reduce_sum(out=ssum, in_=ex, axis=AX.X), [K_('ex')], [K_('ssum')])
                b.recip(ssum, ssum, [K_('ssum')], [K_('ssum')])
                b.ts('dve', comb, ex, ssum, None, ALU.mult, None, [K_('ex'), K_('ssum')], [K_('comb')])
                bk2 = b.bank()
                b.tr(ps[bk2][0:NE, 0:128], comb, self.ident, [K_('comb'), 'cst'], [('ps', bk2)])
                b.cp('act', self.combT[0:NE, tbl * 128:(tbl + 1) * 128], ps[bk2][0:NE, 0:128], [('ps', bk2)],
                     [('combT', tbl)])
        if nxt is not None:
            scb = self.cc(C_SCB, 4).bitcast(BF16)
            for q4 in range(4):
                q = half * 4 + q4
                bk = b.bank()
                for oc in range(6):
                    for kc in range(KC):
                        b.mm(ps[bk][:, oc:oc + 1], wmq[q4][:, kc, oc * 128:(oc + 1) * 128], scb[:, kc:kc + 1],
                             kc == 0, kc == KC - 1, [('wmq', q4, kc)], [('ps', bk)])
                c0 = nxt * 48 + q * 6
                b.tt('dve', self.mod[:, c0:c0 + 6], ps[bk][:, 0:6], self.cc(C_BMODT + c0, 6), ALU.add,
                     [('ps', bk)], [('modn', q)])
            s_ = 1 if half == 0 else 4
            sl_ = self.mod[:, nxt * 48 + s_ * 8:nxt * 48 + s_ * 8 + 8]
            b.ts('dve', sl_, sl_, 1.0, None, ALU.add, None, [('modn', half * 4 + q4) for q4 in range(4)],
                 [('modn', half * 4 + q4) for q4 in range(4)])
        self.moe_off = ar.off - 0
        b.emit()

    def moe_experts(self, l, half):
        b = self.newblk()
        ps = self.ps
        ar = Arena(self.arena, PERSIST)
        hT = ar.bf([8, 1024])
        combT = ar.bf([1024])
        actT = ar.bf([8, 1024])
        WGa = [ar.bf([4, 2048]) for _ in range(2)]
        WGb = ar.bf([4, 2048])
        WD = ar.bf([8, 1024])
        comb_b = ar.bf([1024])
        cmask = ar.bf([1024])
        gb = [ar.f32(TT) for _ in range(3)]
        sb = [ar.f32(TT) for _ in range(3)]
        ub = [ar.f32(TT) for _ in range(3)]
        bgu = ar.f32(512)
        bd = ar.bf([1024])
        ident = self.ident
        onesb = self.cb(7)
        b.dma('sp', bgu, self.b_guT[:, l * 512:(l + 1) * 512], (), ['bgu'])
        b.dma('pool', bd[0:NE, :], self.b_d[l], (), ['bd'])
        bgu3 = bgu.rearrange("p (e c) -> p e c", e=NE)
        b.ts('dve', bgu3[:, :, 8:16], bgu3[:, :, 8:16], 1.0, None, ALU.add, None, ['bgu'], ['bgu'])
        g2 = lambda m: self.modcol(l, 5, m)

        def wgu(e, kc):
            return WGa[e % 2][:, kc, :] if kc < 4 else WGb[:, kc - 4, :]

        def wkey(e, kc):
            return ('wgu', kc, e % 2) if kc < 4 else ('wgu', kc)

        def load_gu_a(e):
            for kc in range(4):
                b.dma('pool', wgu(e, kc), self.w_gu[l, e, kc * 128:(kc + 1) * 128, :], (), [wkey(e, kc)])

        def load_gu_b(e):
            for kc in range(4, KC):
                b.dma('pool', wgu(e, kc), self.w_gu[l, e, kc * 128:(kc + 1) * 128, :], (), [wkey(e, kc)])

        def load_d(e):
            for jc in range(KC):
                b.dma('pool', WD[:, jc, :], self.w_d[l, e, jc * 128:(jc + 1) * 128, :], (), [('wd', jc)])

        def comb_prep(e):
            b.ts('dve', cmask[0:NE, :], combT[0:NE, :], ident[0:NE, e:e + 1], None, ALU.mult, None,
                 [('combT', i) for i in range(8)] + ['cst'], ['cmask'])
            for t in range(2):
                bk = b.bank()
                b.mm(ps[bk][:, :], onesb[0:NE, :], cmask[0:NE, t * TT:(t + 1) * TT], True, True,
                     ['cmask', 'cstb'], [('ps', bk)])
                b.cp('act', comb_b[:, t * TT:(t + 1) * TT], ps[bk][:, :], [('ps', bk)], [('comb_b', t)])

        load_gu_a(0)
        load_gu_b(0)
        load_d(0)
        comb_prep(0)
        it = 0
        pend = None
        NEr = self.ne_run
        for e in range(NEr):
            if e + 1 < NEr:
                load_gu_a(e + 1)
            for j in range(KC):
                for t in range(2):
                    sl = slice(t * TT, (t + 1) * TT)
                    bg = b.bank()
                    bu = b.bank()
                    for kc in range(KC):
                        b.mm(ps[bg][:, :], wgu(e, kc)[:, j * 128:(j + 1) * 128], hT[:, kc, sl], kc == 0, kc == KC - 1,
                             [wkey(e, kc), ('hT', kc, t)], [('ps', bg)])
                    for kc in range(KC):
                        b.mm(ps[bu][:, :], wgu(e, kc)[:, D + j * 128:D + (j + 1) * 128], hT[:, kc, sl], kc == 0,
                             kc == KC - 1, [wkey(e, kc), ('hT', kc, t)], [('ps', bu)])
                    i = it % 3
                    it += 1
                    G, S, U = gb[i], sb[i], ub[i]
                    cg = e * 16 + j
                    cu = e * 16 + 8 + j
                    b.ts('dve', G, ps[bg][:, :], bgu[:, cg:cg + 1], 7.0, ALU.add, ALU.min, [('ps', bg), 'bgu'], [('G', i)])
                    b.ts('dve', U, ps[bu][:, :], bgu[:, cu:cu + 1], 8.0, ALU.add, ALU.min, [('ps', bu), 'bgu'], [('U', i)])
                    b.act(S, G, AF.Sigmoid, [('G', i)], [('S', i)], scale=1.702)
                    b.tt('pool', S, G, S, ALU.mult, [('G', i), ('S', i)], [('S', i)])
                    if pend is not None:
                        pend()

                    def tail(S=S, U=U, i=i, j=j, sl=sl, t=t):
                        b.stt(U, U, -6.0, S, ALU.max, ALU.mult, [('U', i), ('S', i)], [('U', i)])
                        b.tt('pool', actT[:, j, sl], U, comb_b[:, sl], ALU.mult, [('U', i), ('comb_b', t)], [('actT', j, t)])
                    pend = tail
            pend()
            pend = None
            if e + 1 < NEr:
                load_gu_b(e + 1)
            for t in range(2):
                if t == 1 and e + 1 < NEr:
                    comb_prep(e + 1)
                for m in range(KC):
                    sl = slice(t * TT, (t + 1) * TT)
                    gsl = slice((half * 2 + t) * TT, (half * 2 + t + 1) * TT)
                    bk = b.bank()
                    if e == 0:
                        b.mm(ps[bk][:, :], bd[0:NE, m * 128:(m + 1) * 128], combT[0:NE, sl], True, False,
                             ['bd'] + [('combT', t * 4 + q) for q in range(4)], [('ps', bk)])
                    for j in range(KC):
                        b.mm(ps[bk][:, :], WD[:, j, m * 128:(m + 1) * 128], actT[:, j, sl], (j == 0 and e != 0),
                             j == KC - 1, [('wd', j), ('actT', j, t)], [('ps', bk)])
                    xk = ('xT', m, half * 2 + t)
                    b.stt(self.xT[:, m, gsl], ps[bk][:, :], g2(m), self.xT[:, m, gsl], ALU.mult, ALU.add,
                          [('ps', bk), xk, 'mods'], [xk])
            if e + 1 < NEr:
                load_d(e + 1)
        b.emit()
        b = self.newblk()
        ar = Arena(self.arena, PERSIST)
        self.ln(b, ar, [half * 2, half * 2 + 1], lambda kc: self.lncol(l, 1, 0, kc), lambda kc: self.lncol(l, 1, 1, kc),
                self.eps5, lambda kc, t: self.xT[:, kc, t * TT:(t + 1) * TT], lambda kc, t: [('xT', kc, t)])
        b.emit()

    def epilogue(self):
        b = self.newblk()
        ps = self.ps
        ar = Arena(self.arena, PERSIST)
        ys = [ar.f32(512) for _ in range(4)]
        it = 0
        for tb in range(NB):
            for half in range(2):
                bk = b.bank()
                for c in range(4):
                    kc = half * 4 + c
                    b.tr(ps[bk][:, c * 128:(c + 1) * 128], self.xT[:, kc, tb * 128:(tb + 1) * 128], self.ident,
                         [('xT', kc, tb // 4), 'cst'], [('ps', bk)])
                s = ys[it % 4]
                b.cp('act' if it % 2 == 0 else 'dve', s, ps[bk][:, :], [('ps', bk)], [('ys', it % 4)])
                b.dma('sp', self.y[tb * 128:(tb + 1) * 128, half * 512:(half + 1) * 512], s, [('ys', it % 4)], [])
                it += 1
        b.emit(final=True)


    def even_layer(self, l):
        i = l // 2
        A = self.arena
        ps = self.ps
        ident, ones, onesb = self.ident, self.ones, self.cb(7)
        OUTS = self.OUTS

        def region(off, dims):
            a = Arena(A, PERSIST + off)
            return a.bf(dims)

        ufT = region(0, [4, 2048])
        qT = region(4096, [2, 2048])
        kT = region(6144, [2, 2048])
        sgT = region(8192, [4, 2048])
        alrT = region(12288, [2048])
        k_tm = region(13312, [16, 256])
        v_tm = region(15360, [16, 512])
        C_EL = 1712
        elast = self.cc(C_EL, 128)

        self.pre_ln_block(l, OUTS)

        b = self.newblk()
        ar = Arena(A, PERSIST + OUTS)
        hT = ar.bf([8, 2048])
        Wp = [ar.bf([8, 512]) for _ in range(2)]

        def load_piece(p):
            W = Wp[p % 2]
            ncol = 512 if p < 4 else 128
            for kc in range(KC):
                b.dma('pool', W[:, kc, 0:ncol], self.w_in[i, kc * 128:(kc + 1) * 128, p * 512:p * 512 + ncol], (),
                      [('Wp', p % 2, kc)])
            return W

        ev = [0]

        def fm(W, pidx, c0, evac):
            for t in range(NT):
                sl = slice(t * TT, (t + 1) * TT)
                bk = b.bank()
                for kc in range(KC):
                    b.mm(ps[bk][:, :], W[:, kc, c0:c0 + 128], hT[:, kc, sl], kc == 0, kc == KC - 1,
                         [('Wp', pidx % 2, kc), ('hT', kc, t)], [('ps', bk)])
                evac(bk, t, sl)

        def tm(W, pidx, c0, n, dst, dkey):
            for tb in range(NB):
                bk = b.bank()
                for kc in range(KC):
                    b.mm(ps[bk][:, 0:n], hT[:, kc, tb * 128:(tb + 1) * 128], W[:, kc, c0:c0 + n], kc == 0,
                         kc == KC - 1, [('Wp', pidx % 2, kc), ('hT', kc, tb // 4)], [('ps', bk)])
                ev[0] += 1
                b.cp('act' if ev[0] % 2 else 'dve', dst[:, tb, :], ps[bk][:, 0:n], [('ps', bk)], [(dkey, tb)])

        def cp_to(dst3, key):
            def f(bk, t, sl):
                ev[0] += 1
                b.cp('act' if ev[0] % 2 else 'dve', dst3(sl), ps[bk][:, :], [('ps', bk)], [(key, t)])
            return f

        W = load_piece(0)
        W1 = load_piece(1)
        for g in range(4):
            fm(W, 0, g * 128, cp_to(lambda sl, g=g: ufT[:, g, sl], ('ufT', g)))
        W = W1
        for c in range(2):
            def evq(bk, t, sl, c=c):
                b.act(qT[:, c, sl], ps[bk][:, :], AF.Copy, [('ps', bk)], [('qT', c, t)], scale=0.125)
            fm(W, 1, c * 128, evq)
            fm(W, 1, 256 + c * 128, cp_to(lambda sl, c=c: kT[:, c, sl], ('kT', c)))
        tm(W, 1, 256, 256, k_tm, 'k_tm')
        W = load_piece(2)
        tm(W, 2, 0, 512, v_tm, 'v_tm')
        W = load_piece(3)
        for h in range(4):
            def evg(bk, t, sl, h=h):
                b.act(sgT[:, h, sl], ps[bk][:, :], AF.Silu, [('ps', bk)], [('sgT', h, t)])
            fm(W, 3, h * 128, evg)
        W = load_piece(4)
        fm(W, 4, 0, cp_to(lambda sl: alrT[:, sl], 'alrT'))
        b.emit()

        if self.dbg_stop <= 2:
            return self.post_ln_block(l)
        b = self.newblk()
        ar = Arena(A, PERSIST + OUTS)
        AB = ar.bf([16, 4, 256])
        Ctab = ar.bf([16, 256])
        Stab = ar.bf([16, 256])
        cs = ar.bf([256])
        b.dma('sp', cs, self.cs128[:, :], (), ['cs'])
        for tb in range(NB):
            for gp in range(2):
                bk = 4 + (tb * 2 + gp) % 4
                for gg in range(2):
                    g = gp * 2 + gg
                    b.mm(ps[bk][:, gg * 256:(gg + 1) * 256], ufT[:, g, tb * 128:(tb + 1) * 128], cs, True, True,
                         [(('ufT', g), tb // 4), 'cs'], [('ps', bk)])
                b.cp('act' if gp == 0 else 'dve', AB[:, tb, gp * 2:gp * 2 + 2, :],
                     ps[bk][:, :].rearrange("p (a b) -> p a b", a=2), [('ps', bk)], [('AB', tb, gp * 2), ('AB', tb, gp * 2 + 1)])
        dC = self.dftC.rearrange("(tb p) n -> p tb n", p=128)
        dS = self.dftS.rearrange("(tb p) n -> p tb n", p=128)
        for tq in range(8):
            qs = slice(tq * 256, (tq + 1) * 256)
            b.dma('sp', Ctab, dC[:, :, qs], (), ['Ctab'])
            b.dma('sp', Stab, dS[:, :, qs], (), ['Stab'])
            for g in range(4):
                for tb in range(NB):
                    b.mm(ps[g][:, 0:256], AB[:, tb, g, 0:128], Ctab[:, tb, :], tb == 0, False,
                         [('AB', tb, g), 'Ctab'], [('ps', g)])
            for g in range(4):
                for tb in range(NB):
                    b.mm(ps[g][:, 0:256], AB[:, tb, g, 128:256], Stab[:, tb, :], False, tb == NB - 1,
                         [('AB', tb, g), 'Stab'], [('ps', g)])
            for g in range(4):
                b.cp('act' if g % 2 == 0 else 'dve', ufT[:, g, qs], ps[g][:, 0:256], [('ps', g)],
                     [(('ufT', g), tq // 2)])
        b.emit()

        if self.dbg_stop <= 3:
            return self.post_ln_block(l)
        b = self.newblk()
        ar = Arena(A, PERSIST + OUTS)
        q0 = ar.bf([2, 2048])
        k0 = ar.bf([2, 2048])
        kh0 = ar.bf([16, 256])
        logg = ar.bf([16, 512])
        wa = ar.bf([512])
        ba = ar.bf([512])
        tmpa = [ar.f32(512) for _ in range(2)]
        tmpb = [ar.f32(512) for _ in range(2)]
        qd = [q0, qT]
        kd = [k0, kT]
        khd = [kh0, k_tm]
        b.add('dve', lambda e: e.memset(wa, 0.0), (), ['wa'])
        b.add('dve', lambda e: e.memset(ba, 0.0), (), ['ba'])
        b.dma('pool', wa[0:16, 0:256], self.w_a2[i, 0], (), ['wa'])
        b.dma('pool', wa[32:48, 256:512], self.w_a2[i, 1], (), ['wa'])
        b.dma('pool', ba[0:1, :], self.b_a[i].rearrange("(o z) e -> o (z e)", o=1), (), ['ba'])
        one_col = self.ones[:, 0:1]
        E0 = self.cb(0)
        for tb in range(NB):
            bk = b.bank()
            tsl = slice(tb * 128, (tb + 1) * 128)
            b.mm(ps[bk][:, :], E0, ba, True, False, ['cstb', 'ba'], [('ps', bk)])
            b.mm(ps[bk][:, :], alrT[:, tsl], wa, False, True, ['wa'], [('ps', bk)])
            tp = tmpa[tb % 2]
            tk = ('tmpa', tb % 2)
            b.act(tp, ps[bk][:, :], AF.Exp, [('ps', bk)], [tk], scale=-1.0)
            b.act(tp, tp, AF.Ln, [tk], [tk], bias=one_col)
            b.ts('dve', logg[:, tb, :], tp, -1.0 / 16.0, None, ALU.mult, None, [tk], [('logg', tb)])
        itc = 0
        for d in range(2 if self.dbg_sub >= 2 else 0):
            tri = self.cb(1 + d)
            for c in range(2):
                for t in range(NT):
                    sl = slice(t * TT, (t + 1) * TT)
                    bk = b.bank()
                    for q in range(4):
                        tb = t * 4 + q
                        b.mm(ps[bk][:, q * 128:(q + 1) * 128], logg[:, tb, d * 256 + c * 128:d * 256 + (c + 1) * 128],
                             tri, True, True, [('logg', tb), 'cstb'], [('ps', bk)])
                    pos = 63 if d == 0 else 0
                    e0 = (d * 2 + c) * 32 + t * 8
                    b.act(elast[:, e0:e0 + 8], ps[bk][:, :].rearrange("p (a b) -> p a b", a=8)[:, :, pos], AF.Exp,
                          [('ps', bk)], [('elast', d, c, t)])
                    ta = tmpa[itc % 2]
                    tbq = tmpb[itc % 2]
                    ka, kb = ('tmpa', itc % 2), ('tmpb', itc % 2)
                    itc += 1
                    b.act(ta, ps[bk][:, :], AF.Exp, [('ps', bk)], [ka])
                    b.act(tbq, ps[bk][:, :], AF.Exp, [('ps', bk)], [kb], scale=-1.0)
                    b.tt('dve', qd[d][:, c, sl], qT[:, c, sl], ta, ALU.mult, [('qT', c, t), ka], [('qd', d, c, t), ('qT', c, t)] if d == 1 else [('qd', d, c, t)])
                    b.tt('pool', kd[d][:, c, sl], kT[:, c, sl], tbq, ALU.mult, [('kT', c, t), kb], [('kd', d, c, t), ('kT', c, t)] if d == 1 else [('kd', d, c, t)])
            sm = self.cb(3 + d)
            for tb2 in range(NB // 2 if self.dbg_sub >= 3 else 0):
                bk = b.bank()
                for q in range(2):
                    tb = tb2 * 2 + q
                    b.mm(ps[bk][:, q * 256:(q + 1) * 256], sm, logg[:, tb, d * 256:(d + 1) * 256], True, True,
                         [('logg', tb), 'cstb'], [('ps', bk)])
                ta = tmpa[itc % 2]
                ka = ('tmpa', itc % 2)
                itc += 1
                b.act(ta, ps[bk][:, :], AF.Exp, [('ps', bk)], [ka])
                b.tt('dve', khd[d][:, tb2 * 2:tb2 * 2 + 2, :], k_tm[:, tb2 * 2:tb2 * 2 + 2, :],
                     ta.rearrange("p (a b) -> p a b", a=2), ALU.mult, [('k_tm', tb2), ka],
                     [('khd', d, tb2), ('k_tm', tb2)] if d == 1 else [('khd', d, tb2)])
        b.emit()

        if self.dbg_stop <= 4:
            return self.post_ln_block(l)
        b = self.newblk()
        Sbf = Arena(A, PERSIST + 25600).bf([128, 128])
        Mst = Arena(A, PERSIST + 12288).f32v([4, 256])
        for s4 in range(4):
            b.add('dve', lambda e, s4=s4: e.memset(Mst[:, s4, :], 0.0), (), [('M', s4)])
        for d in range(2):
            for p in range(2):
                s4 = d * 2 + p
                b.dma('sp', Mst[0:64, s4, 0:128], self.st0[i, d, 2 * p], (), [('M', s4)])
                b.dma('sp', Mst[64:128, s4, 128:256], self.st0[i, d, 2 * p + 1], (), [('M', s4)])
        for step in range(32):
            for d in range(2):
                c = step if d == 0 else 31 - step
                tb = c // 2
                r0 = (c % 2) * 64
                for p in range(2):
                    s4 = d * 2 + p
                    M = Mst[:, s4, :]
                    sidx = s4 * 32 + c
                    b.cp('act', Sbf[0:64, sidx, :], M[0:64, 0:128], [('M', s4)], [('Sbf', sidx)])
                    b.cp('pool', Sbf[64:128, sidx, :], M[64:128, 128:256], [('M', s4)], [('Sbf', sidx)])
                    bk = b.bank()
                    b.mm(ps[bk][:, 0:256], khd[d][r0:r0 + 64, tb, p * 128:(p + 1) * 128],
                         v_tm[r0:r0 + 64, tb, p * 256:(p + 1) * 256], True, True, [], [('ps', bk)])
                    ei = (d * 2 + p) * 32 + c
                    b.stt(M, M, elast[:, ei:ei + 1], ps[bk][:, 0:256], ALU.mult, ALU.add, [('M', s4), ('ps', bk)],
                          [('M', s4)])
                    boundary = (c % 4 == 3) if d == 0 else (c % 4 == 0)
                    if boundary:
                        n = c // 4
                        b.dma('sp', self.ns[i, d, n, 2 * p], M[0:64, 0:128], [('M', s4)], [])
                        b.dma('sp', self.ns[i, d, n, 2 * p + 1], M[64:128, 128:256], [('M', s4)], [])
                        b.ts('dve', M, M, self.keep_sb[:, n:n + 1], None, ALU.mult, None, [('M', s4)], [('M', s4)])
        b.emit()

        if self.dbg_stop <= 5:
            return self.post_ln_block(l)
        b = self.newblk()
        atm = Arena(A, PERSIST + 12288)
        attm = [atm.bf([256]) for _ in range(2)]
        sqb = atm.f32(256)
        isb = atm.f32(256)
        oT = [Arena(A, PERSIST + 13312).bf([2, 2048]), Arena(A, PERSIST + 23552).bf([2, 2048])]
        bm2 = self.cc(C_CSTB, 512).bitcast(BF16)[:, 5 * 128:7 * 128]
        gcol = self.cc(C_GNG + i, 1)
        ita = 0
        for h in range(4):
            p = h // 2
            r0 = (h % 2) * 64
            for t2 in range(8):
                sl = slice(t2 * 256, (t2 + 1) * 256)
                bo = t2 % 2
                bi = 2 + t2 % 2
                for q in range(2):
                    tb = t2 * 2 + q
                    tsl = slice(tb * 128, (tb + 1) * 128)
                    ba_ = 4 + ita % 2
                    am = attm[ita % 2]
                    ak = ('attm', ita % 2)
                    ita += 1
                    for d in range(2):
                        b.mm(ps[ba_][:, d * 128:(d + 1) * 128], kd[d][r0:r0 + 64, p, tsl], qd[d][r0:r0 + 64, p, tsl],
                             True, True, [], [('ps', ba_)])
                    b.tt('dve', am, ps[ba_][:, 0:256], bm2, ALU.mult, [('ps', ba_)], [ak])
                    oc = ps[bo][:, q * 128:(q + 1) * 128]
                    b.mm(oc, v_tm[:, tb, h * 128:(h + 1) * 128], am[:, 0:128], True, False, [ak], [('ps', bo)])
                    b.mm(oc, v_tm[:, tb, h * 128:(h + 1) * 128], am[:, 128:256], False, True, [ak], [('ps', bo)])
                    for cc in range(2):
                        c = tb * 2 + cc
                        for d in range(2):
                            sidx = (d * 2 + p) * 32 + c
                            b.mm(ps[bi][:, q * 128 + cc * 64:q * 128 + (cc + 1) * 64], Sbf[r0:r0 + 64, sidx, :],
                                 qd[d][r0:r0 + 64, p, c * 64:(c + 1) * 64], d == 0, d == 1, [], [('ps', bi)])
                b.cp('act', isb, ps[bi][:, 0:256], [('ps', bi)], ['isb'])
                b.tt('dve', isb, ps[bo][:, 0:256], isb, ALU.add, [('ps', bo), 'isb'], ['isb'])
                b.act(sqb, isb, AF.Square, ['isb'], ['sqb'])
                b.mm(ps[6][:, 0:256], ones, sqb, True, True, ['sqb'], [('ps', 6)])
                b.act(sqb, ps[6][:, 0:256], AF.Sqrt, [('ps', 6)], ['sqb'], bias=self.eps6, scale=1.0 / 128)
                b.recip(sqb, sqb, ['sqb'], ['sqb'])
                b.tt('dve', isb, isb, sqb, ALU.mult, ['isb', 'sqb'], ['isb'])
                b.stt(oT[p][:, h % 2, sl], isb, gcol, sgT[:, h, sl], ALU.mult, ALU.mult, ['isb'], [('oT', h, t2)])
        b.emit()

        if self.dbg_stop <= 6:
            return self.post_ln_block(l)
        b = self.newblk()
        Wo = Arena(A, PERSIST + 25600).bf([8, 1024])
        for kc in range(KC):
            b.dma('pool', Wo[:, kc, :], self.w_oe[i, kc * 128:(kc + 1) * 128, :], (), [('Wo', kc)])
        for m in range(KC):
            for t in range(NT):
                sl = slice(t * TT, (t + 1) * TT)
                bk = b.bank()
                for kk in range(8):
                    rhs = ufT[:, kk, sl] if kk < 4 else oT[(kk - 4) // 2][:, (kk - 4) % 2, sl]
                    b.mm(ps[bk][:, :], Wo[:, kk, m * 128:(m + 1) * 128], rhs, kk == 0, kk == 7, [('Wo', kk)], [('ps', bk)])
                xk = ('xT', m, t)
                b.stt(self.xT[:, m, sl], ps[bk][:, :], self.modcol(l, 2, m), self.xT[:, m, sl], ALU.mult, ALU.add,
                      [('ps', bk), xk], [xk])
        b.emit()
        self.post_ln_block(l)


    OUTS = 19456

    def pre_ln_block(self, l, base):
        b = self.newblk()
        ar = Arena(self.arena, PERSIST + base)
        hT = ar.bf([8, 2048])
        self.ln(b, ar, range(NT), lambda kc: self.modcol(l, 1, kc), lambda kc: self.modcol(l, 0, kc), self.eps5,
                lambda kc, t: hT[:, kc, t * TT:(t + 1) * TT], lambda kc, t: [('hT', kc, t)], prescale=ALPHA)
        b.emit()

    def post_ln_block(self, l):
        b = self.newblk()
        ar = Arena(self.arena, PERSIST)
        self.ln(b, ar, range(NT), lambda kc: self.lncol(l, 0, 0, kc), lambda kc: self.lncol(l, 0, 1, kc), self.eps5,
                lambda kc, t: self.xT[:, kc, t * TT:(t + 1) * TT], lambda kc, t: [('xT', kc, t)])
        b.emit()

    def odd_layer(self, l):
        i = l // 2
        self.pre_ln_block(l, 0)
        b = self.newblk()
        ps = self.ps
        ar = Arena(self.arena, PERSIST)
        hT = ar.bf([8, 2048])
        qT = ar.bf([8, 2048])
        W = ar.bf([8, 1536])
        kT = ar.bf([2, 2304])
        vS = ar.bf([18, 256])
        rC = ar.f32(512)
        rS = ar.f32(512)
        y32 = ar.f32(512)
        sq32 = ar.f32(512)
        rs = ar.f32(512)
        t1 = ar.f32(512)
        pT = [ar.bf([512]) for _ in range(3)]
        rden = ar.f32(512)
        v32 = [ar.f32(256) for _ in range(2)]
        k32 = [ar.f32(256) for _ in range(2)]
        cks = ar.f32v([2, 256])
        kbc = ar.f32(128)
        ssq = ar.f32(2)
        ident, ones, onesb = self.ident, self.ones, self.cb(7)
        qg = self.cc(C_QNG + i, 1)
        kg = self.cc(C_KNG + i, 1)
        for kc in range(KC):
            b.dma('pool', W[:, kc, :], self.w_qkv[i, kc * 128:(kc + 1) * 128, :], (), [('W', kc)])
        b.dma('sp', cks, self.ck[i].rearrange("(sb p) f -> p sb f", p=128), (), ['cks'])
        b.dma('pool', vS[:, 0:2, :], self.cv[i].rearrange("(sb p) f -> p sb f", p=128), (), [('vS', 0), ('vS', 1)])
        b.dma('sp', kbc, self.kng_bc[:, i * 128:(i + 1) * 128], (), ['kbc'])
        for sbi in range(2):
            bk = 7
            for kv in range(2):
                b.tr(ps[bk][:, kv * 128:(kv + 1) * 128], cks[:, sbi, kv * 128:(kv + 1) * 128], ident,
                     ['cks', 'cst'], [('ps', bk)])
            for kv in range(2):
                b.cp('act', kT[:, kv, sbi * 128:(sbi + 1) * 128], ps[bk][:, kv * 128:(kv + 1) * 128],
                     [('ps', bk)], [('kT', kv, sbi)])
        for tb in range(NB):
            bk = 4 + tb % 4
            tsl = slice(tb * 128, (tb + 1) * 128)
            for kc in range(KC):
                b.mm(ps[bk][:, 0:256], hT[:, kc, tsl], W[:, kc, 1280:1536], kc == 0, kc == KC - 1,
                     [('hT', kc, tb // 4), ('W', kc)], [('ps', bk)])
            for kc in range(KC):
                b.mm(ps[bk][:, 256:512], hT[:, kc, tsl], W[:, kc, 1024:1280], kc == 0, kc == KC - 1,
                     [('hT', kc, tb // 4), ('W', kc)], [('ps', bk)])
            v = v32[tb % 2]
            k = k32[tb % 2]
            b.cp('act', v, ps[bk][:, 0:256], [('ps', bk)], [('v32', tb % 2)])
            b.dma('sp', self.nv[i, tsl, :], v, [('v32', tb % 2)], [])
            b.cp('pool', vS[:, 2 + tb, :], v, [('v32', tb % 2)], [('vS', 2 + tb)])
            for kv in range(2):
                b.act(k[:, kv * 128:(kv + 1) * 128], ps[bk][:, 256 + kv * 128:256 + (kv + 1) * 128], AF.Square,
                      [('ps', bk)], [('k32', tb % 2), ('ssq', kv)], accum_out=ssq[:, kv:kv + 1])
            b.act(ssq, ssq, AF.Sqrt, [('ssq', 0), ('ssq', 1), 'cst'], [('ssq', 0), ('ssq', 1)], bias=self.eps6,
                  scale=1.0 / 128)
            b.recip(ssq, ssq, [('ssq', 0), ('ssq', 1)], [('ssq', 0), ('ssq', 1)])
            for kv in range(2):
                b.stt(k[:, kv * 128:(kv + 1) * 128], ps[bk][:, 256 + kv * 128:256 + (kv + 1) * 128],
                      ssq[:, kv:kv + 1], kbc, ALU.mult, ALU.mult, [('ps', bk), ('ssq', kv), 'kbc'], [('k32', tb % 2)])
            b.dma('sp', self.nk[i, tsl, :], k, [('k32', tb % 2)], [])
        for t in range(NT):
            sl = slice(t * TT, (t + 1) * TT)
            b.dma('sp', rC, self.ropeC[:, sl], (), ['rC'])
            b.dma('sp', rS, self.ropeS[:, sl], (), ['rS'])
            for hc in range(10):
                col0 = hc * 128 if hc < 8 else 1024 + (hc - 8) * 128
                gcol = qg if hc < 8 else kg
                bq = 4 + hc % 2
                for kc in range(KC):
                    b.mm(ps[bq][:, :], W[:, kc, col0:col0 + 128], hT[:, kc, sl], kc == 0, kc == KC - 1,
                         [('W', kc), ('hT', kc, t)], [('ps', bq)])
                b.act(y32, ps[bq][:, :], AF.Identity, [('ps', bq), 'small'], ['y32'], scale=gcol)
                b.act(sq32, ps[bq][:, :], AF.Square, [('ps', bq)], ['sq32'])
                b.mm(ps[6][:, :], ones, sq32, True, True, ['cst', 'sq32'], [('ps', 6)])
                b.mm(ps[7][:, :], self.RT, y32, True, True, ['cst', 'y32'], [('ps', 7)])
                b.act(rs, ps[6][:, :], AF.Sqrt, [('ps', 6), 'cst'], ['rs'], bias=self.eps6, scale=1.0 / 128)
                b.recip(rs, rs, ['rs'], ['rs'])
                b.tt('pool', t1, y32, rC, ALU.mult, ['y32', 'rC'], ['t1'])
                b.tt('dve', sq32, ps[7][:, :], rS, ALU.mult, [('ps', 7), 'rS'], ['sq32'])
                b.tt('pool', t1, t1, sq32, ALU.add, ['t1', 'sq32'], ['t1'])
                if hc < 8:
                    b.tt('dve', qT[:, hc, sl], t1, rs, ALU.mult, ['t1', 'rs'], [('qT', hc, t)])
                else:
                    kv = hc - 8
                    b.tt('dve', kT[:, kv, 256 + t * TT:256 + (t + 1) * TT], t1, rs, ALU.mult, ['t1', 'rs'],
                         [('kT', kv, 2 + t * 4 + q) for q in range(4)])
        scale = 128.0 ** -0.5
        iters = [(h, qp, sbi) for h in range(8) for qp in range(4) for sbi in range(18)]

        def qk(n):
            h, qp, sbi = iters[n]
            kv = h // 4
            q0 = qp * TT
            bs = 4 + n % 3
            for hh in range(2):
                b.mm(ps[bs][:, hh * 256:(hh + 1) * 256], kT[:, kv, sbi * 128:(sbi + 1) * 128],
                     qT[:, h, q0 + hh * 256:q0 + (hh + 1) * 256], True, True,
                     [('kT', kv, sbi), ('qT', h, qp)], [('ps', bs)])
            p = pT[n % 3]
            pk = ('pT', n % 3)
            for hh in range(2):
                c = sbi * 8 + qp * 2 + hh
                b.act(p[:, hh * 256:(hh + 1) * 256], ps[bs][:, hh * 256:(hh + 1) * 256], AF.Exp,
                      [('ps', bs), 'small'], [pk], bias=self.attb_sb[:, c:c + 1], scale=scale)

        def pv(n):
            h, qp, sbi = iters[n]
            kv = h // 4
            q0 = qp * TT
            grp = h * 4 + qp
            bo = grp % 2
            bd = 2 + grp % 2
            p = pT[n % 3]
            pk = ('pT', n % 3)
            b.mm(ps[bo][:, :], vS[:, sbi, kv * 128:(kv + 1) * 128], p, sbi == 0, sbi == 17,
                 [('vS', sbi), pk], [('ps', bo)])
            b.mm(ps[bd][:, :], onesb, p, sbi == 0, sbi == 17, ['cstb', pk], [('ps', bd)])
            if sbi == 17:
                b.recip(rden, ps[bd][:, :], [('ps', bd)], ['rden'])
                b.tt('dve', hT[:, h, q0:q0 + TT], ps[bo][:, :], rden, ALU.mult, [('ps', bo), 'rden'], [('hT', h, qp)])

        qk(0)
        for n in range(len(iters)):
            if n + 1 < len(iters):
                qk(n + 1)
            pv(n)
        for kc in range(KC):
            b.dma('pool', W[:, kc, 0:1024], self.w_o[i, kc * 128:(kc + 1) * 128, :], (), [('W', kc)])
        for m in range(KC):
            for t in range(NT):
                sl = slice(t * TT, (t + 1) * TT)
                bk = 4 + (m * NT + t) % 4
                for hh in range(8):
                    b.mm(ps[bk][:, :], W[:, hh, m * 128:(m + 1) * 128], hT[:, hh, sl], hh == 0, hh == 7,
                         [('W', hh), ('hT', hh, t)], [('ps', bk)])
                xk = ('xT', m, t)
                b.stt(self.xT[:, m, sl], ps[bk][:, :], self.modcol(l, 2, m), self.xT[:, m, sl], ALU.mult, ALU.add,
                      [('ps', bk), xk, 'mods'], [xk])
        b.emit()
        self.post_ln_block(l)


def _bf(a):
    return np.ascontiguousarray(a.astype(ml_dtypes.bfloat16))


def _fm(v):
    v = np.asarray(v, np.float32)
    n = v.shape[-1] // 128
    lead = v.shape[:-1]
    r = v.reshape(lead + (n, 128))
    r = np.moveaxis(r, -1, 0)
    return np.ascontiguousarray(r.reshape(128, -1))


def _const_tables():
    cst = np.zeros((128, 512), np.float32)
    cst[:, 0:128] = np.eye(128, dtype=np.float32)
    RT = np.zeros((128, 128), np.float32)
    for i in range(64):
        RT[2 * i + 1, 2 * i] = -1.0
        RT[2 * i, 2 * i + 1] = 1.0
    cst[:, 128:256] = RT
    cst[:, 256:384] = 1.0
    cst[:, 384] = 1e-5
    cst[:, 385] = 1e-6
    cb = np.zeros((8, 128, 128), np.float32)
    cb[0] = 0.0
    cb[0][0, :] = 1.0
    j = np.arange(128)[:, None]
    i = np.arange(128)[None, :]
    same = (j // 64) == (i // 64)
    cb[1] = same & (j <= i)
    cb[2] = same & (j >= i)
    cb[3] = same & (j > i)
    cb[4] = same & (j < i)
    cb[5] = same & (j <= i)
    cb[6] = same & (j >= i)
    cb[7] = 1.0
    cstb = _bf(np.concatenate(list(cb), axis=1))
    c = np.arange(128)
    ang = 2.0 * np.pi * np.outer(c, c) / 128.0
    cs128 = _bf(np.concatenate([np.cos(ang), np.sin(ang)], axis=1) / np.sqrt(128.0))
    return cst, cstb, cs128


def _dft_tables(tseq):
    t = np.arange(T)
    blk = t // tseq
    loc = (t % tseq).astype(np.float64)
    ang = 2.0 * np.pi * np.outer(loc, loc) / tseq
    same = (blk[:, None] == blk[None, :])
    Cm = np.where(same, np.cos(ang), 0.0) / np.sqrt(tseq)
    Sm = np.where(same, -np.sin(ang), 0.0) / np.sqrt(tseq)
    return _bf(Cm.astype(np.float32)), _bf(Sm.astype(np.float32))


def _rope_tables(latent):
    if not latent:
        return np.ones((128, T), np.float32), np.zeros((128, T), np.float32)
    t = np.arange(T)
    row = (t // 64).astype(np.float32)
    col = (t % 64).astype(np.float32)
    freqs = (10000.0 ** (-np.arange(32, dtype=np.float32) / 32)).astype(np.float32)
    ang = np.concatenate([row[:, None] * freqs, col[:, None] * freqs], axis=-1)
    cos = np.cos(ang).astype(np.float32)
    sin = np.sin(ang).astype(np.float32)
    C = np.repeat(cos.T, 2, axis=0)
    S = np.repeat(sin.T, 2, axis=0)
    return np.ascontiguousarray(C), np.ascontiguousarray(S)


def _attb(latent):
    a = np.zeros((128, 144), np.float32)
    if not latent:
        for sb in range(18):
            for qt in range(8):
                ok = sb >= 2 and ((sb - 2) // 2 == qt)
                a[:, sb * 8 + qt] = 0.0 if ok else NEG_BIG
    return a


def prep_inputs(inp):
    g = lambda k: np.asarray(inp[k], np.float32)
    cst, cstb, cs128 = _const_tables()
    w_in = g('w_in_even')
    w_in_pad = np.zeros((2, D, 2176), np.float32)
    w_in_pad[:, :, 0:2048] = w_in[:, :, 0:2048]
    w_in_pad[:, :, 2048:2064] = w_in[:, :, 2048:2064]
    w_in_pad[:, :, 2080:2096] = w_in[:, :, 2064:2080]
    ln_g, ln_b = g('ln_g'), g('ln_b')
    lnT = _fm(np.stack([ln_g, ln_b], axis=2))
    shared = dict(
        cst=cst, cstb=cstb, cs128=cs128,
        w_mod=g('w_mod'), b_modT=_fm(g('b_mod')), lnT=lnT,
        w_in=w_in_pad, w_a2=g('w_a2'), b_a=g('b_a'), gng=np.ascontiguousarray(g('gla_norm_g').T),
        w_oe=g('w_out_even'), w_qkv=g('w_qkv'), qng=np.ascontiguousarray(g('q_norm_g').T),
        kng=np.ascontiguousarray(g('k_norm_g').T),
        kng_bc=np.ascontiguousarray(np.broadcast_to(g('k_norm_g').reshape(1, 256), (128, 256))),
        w_o=g('w_o'), w_r=g('w_router'), b_r=g('b_router'), w_gu=g('w_gate_up'),
        b_guT=_fm(g('b_gate_up')), w_d=g('w_down'), b_d=g('b_down'),
    )
    lat = dict(zip(('ropeC', 'ropeS'), _rope_tables(True)))
    lat['attb'] = _attb(True)
    lat['keep'] = np.ones((128, 8), np.float32)
    lat['dftC'], lat['dftS'] = _dft_tables(2048)
    pro = dict(zip(('ropeC', 'ropeS'), _rope_tables(False)))
    pro['attb'] = _attb(False)
    pro['keep'] = np.zeros((128, 8), np.float32)
    pro['dftC'], pro['dftS'] = _dft_tables(256)
    xs, xp = g('x_sample'), g('x_prompt')
    maps = []
    for core in range(8):
        m = dict(shared)
        if core < 4:
            m.update(lat)
            m['x'] = np.ascontiguousarray(xs[core])
            m['cT'] = _fm(g('c')[core])
            m['st0'] = np.ascontiguousarray(g('state_gla')[core])
            m['ck'] = np.ascontiguousarray(g('cache_k')[core].reshape(2, 256, 256))
            m['cv'] = np.ascontiguousarray(g('cache_v')[core].reshape(2, 256, 256))
        else:
            grp = (core - 4) % 2
            m.update(pro)
            m['x'] = np.ascontiguousarray(xp[grp * 8:(grp + 1) * 8].reshape(T, D))
            m['cT'] = _fm(g('c_ctx'))
            m['st0'] = np.zeros((2, 2, 4, 64, 128), np.float32)
            m['ck'] = np.zeros((2, 256, 256), np.float32)
            m['cv'] = np.zeros((2, 256, 256), np.float32)
        maps.append(m)
    return maps


_NC_CACHE = {}


def run(inputs, depth=DEPTH, mixers=True, cores=8):
    key = (depth, mixers)
    if key not in _NC_CACHE:
        _NC_CACHE[key] = Builder(depth, mixers).build()
    nc = _NC_CACHE[key]
    maps = prep_inputs(inputs)[:cores]
    res = run_bass_kernel_spmd(nc, maps, core_ids=list(range(cores)))
    return res.results


def kernel(**inputs):
    r = run(inputs)
    y_sample = np.stack([r[c]['y'] for c in range(4)], axis=0)
    y_prompt = np.concatenate([r[4 + gI]['y'].reshape(8, 256, D) for gI in range(2)], axis=0)
    ns = np.concatenate([np.transpose(r[4 + gI]['ns'], (2, 0, 1, 3, 4, 5)) for gI in range(2)], axis=0)
    nk = np.concatenate([np.transpose(r[4 + gI]['nk'].reshape(2, 8, 256, 2, 128), (1, 0, 2, 3, 4)) for gI in range(2)], axis=0)
    nv = np.concatenate([np.transpose(r[4 + gI]['nv'].reshape(2, 8, 256, 2, 128), (1, 0, 2, 3, 4)) for gI in range(2)], axis=0)
    return (np.ascontiguousarray(y_prompt, np.float32), np.ascontiguousarray(y_sample, np.float32),
            np.ascontiguousarray(ns, np.float32), np.ascontiguousarray(nk, np.float32),
            np.ascontiguousarray(nv, np.float32))
```
